# Optimizing a Trainium2 kernel written in Bass

```python
import math
import jax
import jax.numpy as jnp
from jax import lax
import numpy as np

D_MODEL = 1024
BATCH = 8
SEQ = 4096
DEPTH = 4

GLA_HEADS = 4
GLA_DK = D_MODEL // (4 * GLA_HEADS)
GLA_DV = D_MODEL // (2 * GLA_HEADS)
GLA_GATE_RANK = 16
GLA_GATE_TEMP = 16.0
GLA_CHUNK = 64
MOBA_HEADS = 8
MOBA_DH = D_MODEL // (2 * MOBA_HEADS)
MOBA_BLOCK = 256
MOBA_TOPK = 3
MOBA_QCHUNK = 32
CONV_CH = D_MODEL // 2
CONV_K = 3
POOL_CH = D_MODEL // 2
POOL_WINDOWS = (2, 4, 8, 16)
POOL_GROUP = POOL_CH // len(POOL_WINDOWS)
D_FF = 4 * D_MODEL
EPS = 1e-6

GLA_QK = GLA_HEADS * GLA_DK
GLA_V = GLA_HEADS * GLA_DV
MOBA_W = MOBA_HEADS * MOBA_DH
EVEN_SPLITS = (GLA_QK, GLA_QK, GLA_V, GLA_V, GLA_GATE_RANK, MOBA_W, MOBA_W, MOBA_W)
EVEN_IN = sum(EVEN_SPLITS)
ODD_SPLITS = (CONV_CH, CONV_CH, CONV_CH, POOL_CH)
ODD_IN = sum(ODD_SPLITS)
N_EVEN = (DEPTH + 1) // 2
N_ODD = DEPTH // 2

kernel_name = "hybrid_gla_moba_conv_pool_trunk"


def _split(a, sizes):
    return jnp.split(a, np.cumsum(sizes)[:-1].tolist(), axis=-1)


def rmsnorm(x, g):
    xf = x.astype(jnp.float32)
    y = xf * lax.rsqrt(jnp.mean(xf * xf, axis=-1, keepdims=True) + EPS)
    return (y * g.astype(jnp.float32)).astype(x.dtype)


def gla_chunked(q, k, v, log_a):
    B, S, H, dk = q.shape
    dv = v.shape[-1]
    C = GLA_CHUNK
    N = S // C

    def blk(t, d):
        return t.astype(jnp.float32).reshape(B, N, C, H, d).transpose(0, 3, 1, 2, 4)

    q = blk(q, dk) * (dk ** -0.5)
    k = blk(k, dk)
    v = blk(v, dv)
    g = blk(log_a, dk)
    b = jnp.cumsum(g, axis=3)
    q_ = q * jnp.exp(b)
    k_ = k * jnp.exp(-b)
    causal = jnp.tril(jnp.ones((C, C), dtype=bool))
    att = jnp.where(causal, jnp.einsum('bhnid,bhnjd->bhnij', q_, k_), 0.0)
    o_intra = jnp.einsum('bhnij,bhnjv->bhniv', att, v)
    b_last = b[:, :, :, -1:, :]
    k_end = k * jnp.exp(b_last - b)
    d_state = jnp.einsum('bhnjd,bhnjv->bhndv', k_end, v)
    decay = jnp.exp(b_last[:, :, :, 0, :])

    def step(state, inp):
        dcy, ds = inp
        return dcy[..., None] * state + ds, state

    init = jnp.zeros((B, H, dk, dv), jnp.float32)
    _, s_in = lax.scan(step, init, (jnp.moveaxis(decay, 2, 0), jnp.moveaxis(d_state, 2, 0)))
    s_in = jnp.moveaxis(s_in, 0, 2)
    o = o_intra + jnp.einsum('bhnid,bhndv->bhniv', q_, s_in)
    return o.transpose(0, 2, 3, 1, 4).reshape(B, S, H, dv)


def alibi_slopes(n_heads):
    return jnp.exp2(-8.0 * jnp.arange(1, n_heads + 1, dtype=jnp.float32) / n_heads)


def moba_attention(q, k, v):
    B, S, H, dh = q.shape
    BS = MOBA_BLOCK
    QC = MOBA_QCHUNK
    NB = -(-S // BS)
    pad = NB * BS - S
    q = q.transpose(0, 2, 1, 3)
    kp = jnp.pad(k.transpose(0, 2, 1, 3), ((0, 0), (0, 0), (0, pad), (0, 0)))
    vp = jnp.pad(v.transpose(0, 2, 1, 3), ((0, 0), (0, 0), (0, pad), (0, 0)))
    kb = kp.reshape(B, H, NB, BS, dh)
    vb = vp.reshape(B, H, NB, BS, dh)
    kmean = jnp.mean(kb.astype(jnp.float32), axis=3)
    slopes = alibi_slopes(H)
    scale = dh ** -0.5
    k_sel = min(MOBA_TOPK, NB - 1)
    gather = jax.vmap(jax.vmap(lambda blocks, ix: blocks[ix]))

    def chunk(c):
        t0 = c * QC
        blk = t0 // BS
        t = t0 + jnp.arange(QC)
        qc = lax.dynamic_slice_in_dim(q, t0, QC, axis=2)
        ko = lax.dynamic_index_in_dim(kb, blk, axis=2, keepdims=False)
        vo = lax.dynamic_index_in_dim(vb, blk, axis=2, keepdims=False)
        pos_o = blk * BS + jnp.arange(BS)
        dist_o = (t[:, None] - pos_o[None, :]).astype(jnp.float32)
        s_o = jnp.einsum('bhqd,bhpd->bhqp', qc, ko, preferred_element_type=jnp.float32) * scale
        s_o = s_o - slopes[None, :, None, None] * dist_o
        s_o = jnp.where(pos_o[None, :] <= t[:, None], s_o, -jnp.inf)
        if k_sel == 0:
            p = jax.nn.softmax(s_o, axis=-1)
            return jnp.einsum('bhqp,bhpd->bhqd', p.astype(vo.dtype), vo)
        gate = jnp.einsum('bhqd,bhnd->bhqn', qc.astype(jnp.float32), kmean)
        gate = jnp.where(jnp.arange(NB) < blk, gate, -jnp.inf)
        top_v, top_i = lax.top_k(gate, k_sel)
        valid = jnp.isfinite(top_v)
        kg = gather(kb, top_i)
        vg = gather(vb, top_i)
        pos_g = top_i[..., None] * BS + jnp.arange(BS)
        dist_g = (t[:, None, None] - pos_g).astype(jnp.float32)
        s_g = jnp.einsum('bhqd,bhqkpd->bhqkp', qc, kg, preferred_element_type=jnp.float32) * scale
        s_g = s_g - slopes[None, :, None, None, None] * dist_g
        s_g = jnp.where(valid[..., None], s_g, -jnp.inf)
        logits = jnp.concatenate([s_g.reshape(B, H, QC, k_sel * BS), s_o], axis=-1)
        p = jax.nn.softmax(logits, axis=-1)
        p_g = p[..., :k_sel * BS].reshape(B, H, QC, k_sel, BS).astype(vg.dtype)
        p_o = p[..., k_sel * BS:].astype(vo.dtype)
        return (jnp.einsum('bhqkp,bhqkpd->bhqd', p_g, vg)
                + jnp.einsum('bhqp,bhpd->bhqd', p_o, vo))

    outs = lax.map(chunk, jnp.arange(S // QC))
    return outs.transpose(1, 0, 3, 2, 4).reshape(B, S, H * dh)


def mix_even(h, w_in, w_gate2, b_gate, gla_norm, q_norm, k_norm, w_o):
    B, S, _ = h.shape
    proj = h @ w_in
    qa, ka, va, ra, glr, qb, kb, vb = _split(proj, EVEN_SPLITS)
    log_a = jax.nn.log_sigmoid((glr @ w_gate2 + b_gate).astype(jnp.float32)) / GLA_GATE_TEMP
    oa = gla_chunked(qa.reshape(B, S, GLA_HEADS, GLA_DK), ka.reshape(B, S, GLA_HEADS, GLA_DK),
                     va.reshape(B, S, GLA_HEADS, GLA_DV), log_a.reshape(B, S, GLA_HEADS, GLA_DK))
    oa = rmsnorm(oa, gla_norm).reshape(B, S, GLA_V) * jax.nn.silu(ra.astype(jnp.float32))
    qb = rmsnorm(qb.reshape(B, S, MOBA_HEADS, MOBA_DH), q_norm)
    kb = rmsnorm(kb.reshape(B, S, MOBA_HEADS, MOBA_DH), k_norm)
    ob = moba_attention(qb, kb, vb.reshape(B, S, MOBA_HEADS, MOBA_DH))
    o = jnp.concatenate([oa.astype(h.dtype), ob.astype(h.dtype)], axis=-1)
    return o @ w_o


def causal_dwconv(z, w):
    C = z.shape[-1]
    return lax.conv_general_dilated(
        z, w.reshape(CONV_K, 1, C).astype(z.dtype), window_strides=(1,),
        padding=[(CONV_K - 1, 0)], dimension_numbers=('NWC', 'WIO', 'NWC'),
        feature_group_count=C)


def multiscale_pool(u, pool_w, pool_scale):
    B, S, _ = u.shape
    uf = u.astype(jnp.float32)
    cs = jnp.pad(jnp.cumsum(uf, axis=1), ((0, 0), (1, 0), (0, 0)))
    t = jnp.arange(S)
    groups = []
    for g, w in enumerate(POOL_WINDOWS):
        sl = slice(g * POOL_GROUP, (g + 1) * POOL_GROUP)
        lo = jnp.maximum(t + 1 - w, 0)
        cnt = (t + 1 - lo).astype(jnp.float32)[None, :, None]
        mean = (cs[:, 1:, sl] - cs[:, lo, sl]) / cnt
        groups.append(mean - uf[:, :, sl])
    pooled = jnp.stack(groups, axis=2)
    y = jnp.einsum('bsgc,gcd->bsgd', pooled, pool_w.astype(jnp.float32)).reshape(B, S, POOL_CH)
    return (y * pool_scale.astype(jnp.float32)).astype(u.dtype)


def mix_odd(h, w_in, conv_w, pool_w, pool_scale, w_o):
    proj = h @ w_in
    bg, cg, xc, u = _split(proj, ODD_SPLITS)
    oc = bg * causal_dwconv(cg * xc, conv_w)
    od = multiscale_pool(u, pool_w, pool_scale)
    return jnp.concatenate([oc, od], axis=-1) @ w_o


def mlp(h, w1, w2):
    a = jax.nn.relu(h @ w1)
    return (a * a) @ w2


def setup_inputs(seed: int = 0) -> dict:
    key = jax.random.key(seed)
    ks = jax.random.split(key, 16)
    f32 = jnp.float32

    def nrm(k, shape, fan_in):
        return jax.random.normal(k, shape, f32) * (fan_in ** -0.5)

    def gain(k, shape):
        return 1.0 + 0.02 * jax.random.normal(k, shape, f32)

    return {
        "x": jax.random.normal(ks[0], (BATCH, SEQ, D_MODEL), f32),
        "norm_mix": gain(ks[1], (DEPTH, D_MODEL)),
        "norm_mlp": gain(ks[2], (DEPTH, D_MODEL)),
        "w_o": nrm(ks[3], (DEPTH, D_MODEL, D_MODEL), D_MODEL),
        "w1": nrm(ks[4], (DEPTH, D_MODEL, D_FF), D_MODEL),
        "w2": nrm(ks[5], (DEPTH, D_FF, D_MODEL), D_FF),
        "even_w_in": nrm(ks[6], (N_EVEN, D_MODEL, EVEN_IN), D_MODEL),
        "gla_w_gate2": nrm(ks[7], (N_EVEN, GLA_GATE_RANK, GLA_QK), GLA_GATE_RANK),
        "gla_b_gate": 0.1 * jax.random.normal(ks[8], (N_EVEN, GLA_QK), f32),
        "gla_out_norm": gain(ks[9], (N_EVEN, GLA_DV)),
        "moba_q_norm": gain(ks[10], (N_EVEN, MOBA_DH)),
        "moba_k_norm": gain(ks[11], (N_EVEN, MOBA_DH)),
        "odd_w_in": nrm(ks[12], (N_ODD, D_MODEL, ODD_IN), D_MODEL),
        "conv_w": nrm(ks[13], (N_ODD, CONV_K, CONV_CH), CONV_K),
        "pool_w": nrm(ks[14], (N_ODD, len(POOL_WINDOWS), POOL_GROUP, POOL_GROUP), POOL_GROUP),
        "pool_scale": gain(ks[15], (N_ODD, POOL_CH)),
    }


def reference(x, norm_mix, norm_mlp, w_o, w1, w2, even_w_in, gla_w_gate2, gla_b_gate,
              gla_out_norm, moba_q_norm, moba_k_norm, odd_w_in, conv_w, pool_w, pool_scale):
    for i in range(DEPTH):
        j = i // 2
        h = rmsnorm(x, norm_mix[i])
        if i % 2 == 0:
            x = x + mix_even(h, even_w_in[j], gla_w_gate2[j], gla_b_gate[j], gla_out_norm[j],
                             moba_q_norm[j], moba_k_norm[j], w_o[i])
        else:
            x = x + mix_odd(h, odd_w_in[j], conv_w[j], pool_w[j], pool_scale[j], w_o[i])
        h = rmsnorm(x, norm_mlp[i])
        x = x + mlp(h, w1[i], w2[i])
    return x
```

```python
from contextlib import ExitStack

import numpy as np
import concourse.bass as bass
import concourse.mybir as mybir
from concourse.bass_utils import run_bass_kernel_spmd

F32 = mybir.dt.float32
BF16 = mybir.dt.bfloat16
AF = mybir.ActivationFunctionType
ALU = mybir.AluOpType
AX = mybir.AxisListType

D = 1024
SEQ = 4096
DEPTH = 4
DFF = 4096
EPS = 1e-6
NEG = -30000.0


class _Op:
    __slots__ = ("eng", "fn", "deps", "dma", "signal", "seq", "sem", "idx")


class _EngProxy:
    def __init__(self, sched, name):
        self._s = sched
        self._n = name

    def __getattr__(self, meth):
        def call(**kw):
            return self._s._record(self._n, meth, kw)
        return call


WRITE_KEYS = ("out", "accum_out", "ap")
EPOCH = 12000
NDMASEM = 40
NSW = 20


class Sched:
    def __init__(self, nc):
        self.nc = nc
        self.engs = {"pe": nc.tensor, "act": nc.scalar, "dve": nc.vector,
                     "pool": nc.gpsimd, "sp": nc.sync}
        self.pe = _EngProxy(self, "pe")
        self.act = _EngProxy(self, "act")
        self.dve = _EngProxy(self, "dve")
        self.pool = _EngProxy(self, "pool")
        self.sp = _EngProxy(self, "sp")
        self.ops = []
        self.emitted = 0
        self.recs = {}
        self.untracked = set()
        self._stack = []
        self.cnt = {e: 0 for e in self.engs}
        self.engsems = {}
        self.dmasems = None
        self.dmacnt = [0] * NDMASEM
        self.dmalast = [None] * NDMASEM
        self.ndma = 0
        self.nsw = 0
        self.waited = {e: {} for e in self.engs}
        self.last_op = {}
        self.open_dmas = []
        self.barrier_deps = None
        self.barrier_done = set()

    def keep(self, cm):
        t = cm.__enter__()
        self._stack.append(cm)
        return t

    def _sem(self, name):
        return self.keep(self.nc.semaphore(name))

    @staticmethod
    def _box(ap):
        if ap.tensor.name.startswith("pb"):
            return (0, 128, 0, 1 << 30)
        shp = ap.tensor.shape
        pstride = 1
        for d in shp[1:]:
            pstride *= int(d)
        off = int(ap.offset)
        p0 = off // pstride
        f0 = off % pstride
        pe = 0
        fe = 0
        for step, cnt in ap.ap:
            step = int(step)
            cnt = int(cnt)
            if cnt <= 1:
                continue
            if step >= pstride and step % pstride == 0:
                pe += (cnt - 1) * (step // pstride)
            else:
                fe += (cnt - 1) * step
        return (p0, p0 + pe + 1, f0, f0 + fe + 1)

    def _record(self, eng, meth, kw):
        idx = len(self.ops)
        op = _Op()
        op.eng = eng
        op.idx = idx
        op.dma = meth in ("dma_start", "dma_start_transpose")
        op.signal = False
        op.seq = None
        op.sem = None
        deps = set()
        if self.barrier_deps is not None and eng not in self.barrier_done:
            deps.update(self.barrier_deps)
            self.barrier_done.add(eng)
        engkey = ("dma", idx) if op.dma else eng
        reads = []
        writes = []
        for k, v in kw.items():
            if isinstance(v, bass.AP):
                (writes if k in WRITE_KEYS else reads).append(v)
        for ap in reads:
            nm = ap.tensor.name
            if nm in self.untracked:
                continue
            b = self._box(ap)
            lst = self.recs.get(nm, ())
            new = []
            for r in lst:
                ov = not (r[1] <= b[0] or b[1] <= r[0] or r[3] <= b[2] or b[3] <= r[2])
                if ov and r[5]:
                    deps.add(r[4])
                if (not r[5]) and r[6] == engkey and b[0] <= r[0] and r[1] <= b[1] and b[2] <= r[2] and r[3] <= b[3]:
                    continue
                new.append(r)
            new.append((b[0], b[1], b[2], b[3], idx, False, engkey))
            self.recs[nm] = new
        for ap in writes:
            nm = ap.tensor.name
            b = self._box(ap)
            lst = self.recs.get(nm, ())
            new = []
            for r in lst:
                ov = not (r[1] <= b[0] or b[1] <= r[0] or r[3] <= b[2] or b[3] <= r[2])
                if ov:
                    deps.add(r[4])
                    if b[0] <= r[0] and r[1] <= b[1] and b[2] <= r[2] and r[3] <= b[3]:
                        continue
                new.append(r)
            new.append((b[0], b[1], b[2], b[3], idx, True, engkey))
            self.recs[nm] = new
        deps.discard(idx)
        op.deps = deps
        op.fn = (meth, kw)
        self.ops.append(op)
        if op.dma:
            self.open_dmas.append(op)
        else:
            self.last_op[eng] = op
        return op

    def _do_wait(self, eng, sem, key, val):
        w = self.waited[eng]
        if w.get(key, 0) >= val:
            return
        w[key] = val
        self.engs[eng].wait_ge(sem, val)

    def flush(self):
        ops = self.ops
        if self.dmasems is None:
            self.dmasems = [self._sem(f"dq{i}") for i in range(NDMASEM)]
        pend = ops[self.emitted:]
        for op in pend:
            for j in op.deps:
                d = ops[j]
                if (not d.dma) and d.eng == "pe" and op.eng == "pe" and not op.dma:
                    continue
                d.signal = True
        for op in pend:
            eng = op.eng
            e = self.engs[eng]
            for j in sorted(op.deps):
                d = ops[j]
                if (not d.dma) and d.eng == "pe" and eng == "pe" and not op.dma:
                    continue
                self._do_wait(eng, d.sem[0], d.sem[1], d.seq)
            if op.dma:
                op.signal = True
                if eng == "pool":
                    si = self.nsw % NSW
                    self.nsw += 1
                else:
                    si = NSW + self.ndma % (NDMASEM - NSW)
                    self.ndma += 1
                prev = self.dmalast[si]
                if prev is not None:
                    self._do_wait(eng, prev.sem[0], prev.sem[1], prev.seq)
                self.dmacnt[si] += 16
                op.sem = (self.dmasems[si], ("d", si))
                op.seq = self.dmacnt[si]
                self.dmalast[si] = op
            elif op.signal:
                self.cnt[eng] += 1
                ep = self.cnt[eng] // EPOCH
                if self.cnt[eng] - ep * EPOCH == 0:
                    self.cnt[eng] += 1
                key = (eng, ep)
                if key not in self.engsems:
                    self.engsems[key] = self._sem(f"s_{eng}_{ep}")
                op.sem = (self.engsems[key], key)
                op.seq = self.cnt[eng] - ep * EPOCH
            meth, kw = op.fn
            ins = getattr(e, meth)(**kw)
            if op.signal:
                ins.then_inc(op.sem[0], 16 if op.dma else 1)
            op.fn = None
        self.emitted = len(ops)

    def barrier(self):
        deps = []
        for eng, op in self.last_op.items():
            op.signal = True
            deps.append(op.idx)
        self.flush()
        for si in range(NDMASEM):
            if self.dmalast[si] is not None:
                deps.append(self.dmalast[si].idx)
        self.barrier_deps = deps
        self.barrier_done = set()
        self.recs = {}
        self.open_dmas = []

    def finish(self):
        self.barrier()
        for j in self.barrier_deps:
            d = self.ops[j]
            self._do_wait("sp", d.sem[0], d.sem[1], d.seq)


def alibi_slopes():
    return [2.0 ** (-(h + 1)) for h in range(8)]


def host_constants():
    c = {}
    c["ident"] = np.eye(128, dtype=np.float32)
    c["onesm"] = np.ones((128, 128), np.float32)
    jj = np.arange(128)[:, None]
    ii = np.arange(128)[None, :]
    c["triu"] = np.where(jj <= ii, -1.0 / 16.0, 0.0).astype(np.float32)
    c["tril"] = np.where(jj > ii, -1.0 / 16.0, 0.0).astype(np.float32)
    c["cmask"] = np.tile(np.where(jj <= ii, 1.0, 0.0).astype(np.float32), (1, 4))
    c["blockones"] = np.kron(np.eye(2, dtype=np.float32), np.ones((64, 64), np.float32))
    oh = np.zeros((128, 16, 128), np.float32)
    for n in range(16):
        oh[n, n, :] = 1.0
        oh[15, n, :] = 1.0
    c["onehot"] = oh
    sl = alibi_slopes()
    own = np.zeros((128, 8, 2, 256), np.float32)
    kl = np.arange(128)[:, None]
    ql = np.arange(256)[None, :]
    for h in range(8):
        for j in range(2):
            pk = j * 128 + kl
            dist = (ql - pk).astype(np.float32)
            own[:, h, j, :] = np.where(pk <= ql, -sl[h] * dist, NEG)
    c["ownmask"] = own
    ab = np.zeros((128, 8, 32), np.float32)
    for h in range(8):
        for dl in range(32):
            ab[:, h, dl] = sl[h] * (np.arange(128) - dl * 128.0)
    c["alibib"] = ab
    aq = np.zeros((128, 2, 8), np.float32)
    for ts in range(2):
        for h in range(8):
            aq[:, ts, h] = -sl[h] * (ts * 128 + np.arange(128))
    c["alibiq"] = aq
    return c


CONST_SHAPES = {
    "ident": [128, 128], "onesm": [128, 128], "triu": [128, 128], "tril": [128, 128],
    "cmask": [128, 512], "blockones": [128, 128], "onehot": [128, 16, 128],
    "ownmask": [128, 8, 2, 256], "alibib": [128, 8, 32], "alibiq": [128, 2, 8],
}

PARAM_SHAPES = {
    "gains": [128, 8, 8],
    "w_o": [4, 1024, 1024], "w1": [4, 1024, 4096], "w2": [4, 4096, 1024],
    "even_w_in": [2, 1024, 3088], "odd_w_in": [2, 1024, 2048],
    "wg_aug": [2, 17, 256], "gla_gain": [2, 128, 1], "qn": [2, 128, 1], "kn": [2, 128, 1],
    "convw": [128, 2, 4, 3], "poolscale": [128, 2, 4], "poolw": [2, 128, 4, 128],
}


class Builder:
    def __init__(self, plan, seq=SEQ):
        self.plan = plan
        self.seq = seq
        nc = bass.Bass("TRN2", target_bir_lowering=False)
        self.nc = nc
        self.S = Sched(nc)
        S = self.S
        self.dr = {}
        for nm, shp in list(CONST_SHAPES.items()) + list(PARAM_SHAPES.items()):
            self.dr[nm] = nc.dram_tensor(nm, shp, F32, kind="ExternalInput").ap()
            S.untracked.add(nm)
        self.xT = nc.dram_tensor("xT", [D, seq], F32, kind="ExternalInput").ap()
        S.untracked.add("xT")
        self.yT = nc.dram_tensor("yT", [D, seq], F32, kind="ExternalOutput").ap()
        self.xs = nc.dram_tensor("xs", [D, seq], F32, kind="Internal").ap()
        self.PB = [S.keep(nc.psum_tensor(f"pb{i}", [128, 512], F32)) for i in range(7)]
        self.PH = S.keep(nc.psum_tensor("pbh", [128, 1024], BF16))
        self.ones_bf = S.keep(nc.sbuf_tensor("ones_bf", [128, 128], BF16))
        self.gains = S.keep(nc.sbuf_tensor("gains_sb", [128, 8, 8], F32))
        self.epsc = S.keep(nc.sbuf_tensor("epsc", [128, 1], F32))
        S.pool.dma_start(out=self.ones_bf[:], in_=self.dr["onesm"])
        S.sp.dma_start(out=self.gains[:], in_=self.dr["gains"])
        S.dve.memset(ap=self.epsc[:], constant=EPS)
        self._bank = 0
        n = len(plan)
        for i, (kind, L) in enumerate(plan):
            src = self.xT if i == 0 else self.xs
            dst = self.yT if i == n - 1 else self.xs
            if kind == "mlp":
                self.mlp_phase(L, src, dst, i)
            elif kind == "odd":
                self.odd_phase(L, src, dst, i)
            elif kind == "even":
                self.even_phase(L, src, dst, i)
            else:
                raise ValueError(kind)
        S.finish()

    def bank(self, lo=0, hi=7):
        b = self.PB[lo + self._bank % (hi - lo)]
        self._bank += 1
        return b

    def load_x(self, XT, src, t0, T):
        S = self.S
        v = src[:, t0:t0 + T].rearrange("(c p) t -> p c t", p=128)
        for c in range(8):
            S.sp.dma_start(out=XT[:, c, 0:T], in_=v[:, c, :])

    def load_x_chunk(self, XT, src, t0, T, c):
        v = src[:, t0:t0 + T].rearrange("(c p) t -> p c t", p=128)
        self.S.sp.dma_start(out=XT[:, c, 0:T], in_=v[:, c, :])

    def store_x_chunk(self, XT, dst, t0, T, c):
        v = dst[:, t0:t0 + T].rearrange("(c p) t -> p c t", p=128)
        self.S.sp.dma_start(out=v[:, c, :], in_=XT[:, c, 0:T])

    def rmsnorm_T(self, XT, HT, SQ, RS, gi, T):
        S = self.S
        pb = self.bank()
        for c in range(8):
            sq = SQ[c % 2]
            S.act.activation(out=sq[:, 0:T], in_=XT[:, c, 0:T], func=AF.Square)
            S.pe.matmul(out=pb[:, 0:T], lhsT=self.ones_bf[:], rhs=sq[:, 0:T], start=(c == 0), stop=(c == 7))
        S.act.activation(out=RS[:, 0:T], in_=pb[:, 0:T], func=AF.Ln, bias=self.epsc[:], scale=1.0 / D)
        S.act.activation(out=RS[:, 0:T], in_=RS[:, 0:T], func=AF.Exp, scale=-0.5)
        for c in range(8):
            S.dve.scalar_tensor_tensor(out=HT[:, c, 0:T], in0=XT[:, c, 0:T], scalar=self.gains[:, gi, c:c + 1],
                                       in1=RS[:, 0:T], op0=ALU.mult, op1=ALU.mult)

    def mlp_phase(self, L, src, dst, pid):
        S = self.S
        nc = self.nc
        T = 512
        NT = self.seq // T
        with ExitStack() as es:
            def sb(name, shape, dt):
                return es.enter_context(nc.sbuf_tensor(f"{name}_{pid}", list(shape), dt))
            W1 = sb("w1s", [128, 8, DFF], BF16)
            W2 = sb("w2s", [128, 32, D], BF16)
            XTs = [sb(f"xt{i}", [128, 8, T], F32) for i in range(2)]
            HTs = [sb(f"ht{i}", [128, 8, T], BF16) for i in range(2)]
            AT = sb("at", [128, 16, T], BF16)
            SQ8 = sb("sq8", [128, 8, T], BF16)
            SQ = [sb(f"sq{i}", [128, T], BF16) for i in range(2)]
            RS = sb("rs", [128, T], F32)
            RL = [sb(f"rl{i}", [128, T], BF16) for i in range(2)]
            w1 = self.dr["w1"]
            w2 = self.dr["w2"]
            self.load_x(XTs[0], src, 0, T)
            for kc in range(8):
                S.pool.dma_start(out=W1[:, kc, :], in_=w1[L, kc * 128:(kc + 1) * 128, :])
            for f4 in range(8):
                S.pool.dma_start(out=W2[:, f4 * 4:(f4 + 1) * 4, :],
                                 in_=w2[L, f4 * 512:(f4 + 1) * 512, :].rearrange("(a p) n -> p a n", p=128))
            self.rmsnorm_T(XTs[0], HTs[0], SQ, RS, 4 + L, T)
            for it in range(NT):
                t0 = it * T
                XT = XTs[it % 2]
                HT = HTs[it % 2]
                nxt = it + 1 < NT
                if nxt:
                    XN = XTs[(it + 1) % 2]
                    HN = HTs[(it + 1) % 2]
                    self.load_x(XN, src, t0 + T, T)
                for half in range(2):
                    for f in range(16):
                        fc = half * 16 + f
                        pb = self.bank()
                        for kc in range(8):
                            S.pe.matmul(out=pb[:, :], lhsT=W1[:, kc, fc * 128:(fc + 1) * 128], rhs=HT[:, kc, :],
                                        start=(kc == 0), stop=(kc == 7))
                        rl = RL[fc % 2]
                        S.act.activation(out=rl[:, :], in_=pb[:, :], func=AF.Relu)
                        S.dve.tensor_tensor(out=AT[:, f, :], in0=rl[:, :], in1=rl[:, :], op=ALU.mult)
                        if nxt and half == 0 and f == 2:
                            for c in range(8):
                                S.act.activation(out=SQ8[:, c, :], in_=XN[:, c, :], func=AF.Square)
                        if nxt and half == 1 and f == 2:
                            pn = self.bank()
                            for c in range(8):
                                S.pe.matmul(out=pn[:, :], lhsT=self.ones_bf[:], rhs=SQ8[:, c, :], start=(c == 0), stop=(c == 7))
                            S.act.activation(out=RS[:, :], in_=pn[:, :], func=AF.Ln, bias=self.epsc[:], scale=1.0 / D)
                            S.act.activation(out=RS[:, :], in_=RS[:, :], func=AF.Exp, scale=-0.5)
                            for c in range(8):
                                S.dve.scalar_tensor_tensor(out=HN[:, c, :], in0=XN[:, c, :], scalar=self.gains[:, 4 + L, c:c + 1],
                                                           in1=RS[:, :], op0=ALU.mult, op1=ALU.mult)
                    for dc in range(8):
                        pb = self.bank()
                        for f in range(16):
                            fc = half * 16 + f
                            S.pe.matmul(out=pb[:, :], lhsT=W2[:, fc, dc * 128:(dc + 1) * 128], rhs=AT[:, f, :],
                                        start=(f == 0), stop=(f == 15))
                        S.dve.tensor_tensor(out=XT[:, dc, :], in0=XT[:, dc, :], in1=pb[:, :], op=ALU.add)
                        if half == 1:
                            self.store_x_chunk(XT, dst, t0, T, dc)
            S.barrier()

    def odd_phase(self, L, src, dst, pid):
        S = self.S
        nc = self.nc
        j = L // 2
        T = 512
        NT = self.seq // T
        WIN = (2, 4, 8, 16)
        W_ = T + 16
        with ExitStack() as es:
            def sb(name, shape, dt):
                return es.enter_context(nc.sbuf_tensor(f"{name}_{pid}", list(shape), dt))
            WI = sb("wi", [128, 8, 2048], BF16)
            WO = sb("wo", [128, 8, D], BF16)
            PW = sb("pw", [128, 4, 128], BF16)
            CW = sb("cw", [128, 4, 3], F32)
            PSC = sb("psc", [128, 4], F32)
            XTs = [sb(f"xt{i}", [128, 8, T], F32) for i in range(2)]
            HTs = [sb(f"ht{i}", [128, 8, T], BF16) for i in range(2)]
            OC = sb("oc", [128, 8, T], BF16)
            SQ = [sb(f"sq{i}", [128, T], BF16) for i in range(2)]
            RS = sb("rs", [128, T], F32)
            CG = [sb(f"cg{i}", [128, T], F32) for i in range(2)]
            ZB = sb("zb", [128, 4, T + 2], F32)
            AC = [sb(f"ac{i}", [128, T], F32) for i in range(2)]
            UB = sb("ub", [128, 4, T + 16], F32)
            SA = [sb(f"sa{i}", [128, T + 16], F32) for i in range(4)]
            PL = [sb(f"pl{i}", [128, T], BF16) for i in range(2)]
            ICN = sb("icn", [128, 4, 16], F32)
            TMP = sb("tmp16", [128, 16], F32)
            self.load_x(XTs[0], src, 0, T)
            wi = self.dr["odd_w_in"]
            wo = self.dr["w_o"]
            for kc in range(8):
                S.pool.dma_start(out=WI[:, kc, :], in_=wi[j, kc * 128:(kc + 1) * 128, :])
            S.pool.dma_start(out=WO[:, :, :], in_=wo[L].rearrange("(a p) n -> p a n", p=128))
            S.pool.dma_start(out=PW[:, :, :], in_=self.dr["poolw"][j])
            S.sp.dma_start(out=CW[:, :, :], in_=self.dr["convw"][:, j])
            S.sp.dma_start(out=PSC[:, :], in_=self.dr["poolscale"][:, j])
            S.dve.memset(ap=ZB[:, :, 0:2], constant=0.0)
            S.dve.memset(ap=UB[:, :, 0:16], constant=0.0)
            for g, w in enumerate(WIN):
                S.dve.memset(ap=ICN[:, g, :], constant=1.0 / w)
                for t in range(w - 1):
                    S.dve.memset(ap=ICN[:, g, t:t + 1], constant=1.0 / (t + 1))
            self.rmsnorm_T(XTs[0], HTs[0], SQ, RS, L, T)
            for it in range(NT):
                t0 = it * T
                XT = XTs[it % 2]
                HT = HTs[it % 2]
                if it + 1 < NT:
                    self.load_x(XTs[(it + 1) % 2], src, t0 + T, T)

                def proj(col):
                    pb = self.bank()
                    for kc in range(8):
                        S.pe.matmul(out=pb[:, :], lhsT=WI[:, kc, col * 128:(col + 1) * 128], rhs=HT[:, kc, :],
                                    start=(kc == 0), stop=(kc == 7))
                    return pb

                def pool_head(g):
                    w = WIN[g]
                    p_u = proj(12 + g)
                    S.act.copy(out=UB[:, g, 16:W_], in_=p_u[:, :])
                    cur = UB[:, g, :]
                    sh = 1
                    lo = 0
                    k = 0
                    nst = {2: 1, 4: 2, 8: 3, 16: 4}[w]
                    while sh < w:
                        nlo = lo + sh
                        o = SA[2 + g % 2] if k == nst - 1 else SA[k % 2]
                        S.pool.tensor_tensor(out=o[:, nlo:W_], in0=cur[:, nlo:W_], in1=cur[:, nlo - sh:W_ - sh], op=ALU.add)
                        cur = o
                        lo = nlo
                        sh *= 2
                        k += 1
                    return cur

                def pool_tail(g, cur):
                    w = WIN[g]
                    pl = PL[g % 2]
                    S.dve.scalar_tensor_tensor(out=pl[:, :], in0=cur[:, 16:W_], scalar=1.0 / w, in1=UB[:, g, 16:W_],
                                               op0=ALU.mult, op1=ALU.subtract)
                    if it == 0:
                        S.dve.tensor_tensor(out=TMP[:, :], in0=cur[:, 16:32], in1=ICN[:, g, :], op=ALU.mult)
                        S.dve.tensor_tensor(out=pl[:, 0:16], in0=TMP[:, :], in1=UB[:, g, 16:32], op=ALU.subtract)
                    S.pool.tensor_copy(out=UB[:, g, 0:16], in_=UB[:, g, T:T + 16])
                    pb = self.bank()
                    S.pe.matmul(out=pb[:, :], lhsT=PW[:, g, :], rhs=pl[:, :], start=True, stop=True)
                    S.act.activation(out=OC[:, 4 + g, :], in_=pb[:, :], func=AF.Copy, scale=PSC[:, g:g + 1])

                def conv_chunk(c):
                    cg = CG[c % 2]
                    ac = AC[c % 2]
                    p_cg = proj(4 + c)
                    S.act.copy(out=cg[:, :], in_=p_cg[:, :])
                    p_xc = proj(8 + c)
                    S.dve.tensor_tensor(out=ZB[:, c, 2:T + 2], in0=p_xc[:, :], in1=cg[:, :], op=ALU.mult)
                    p_bg = proj(c)
                    S.dve.tensor_scalar(out=ac[:, :], in0=ZB[:, c, 2:T + 2], scalar1=CW[:, c, 2:3], scalar2=None,
                                        op0=ALU.mult)
                    S.dve.scalar_tensor_tensor(out=ac[:, :], in0=ZB[:, c, 1:T + 1], scalar=CW[:, c, 1:2], in1=ac[:, :],
                                               op0=ALU.mult, op1=ALU.add)
                    S.dve.scalar_tensor_tensor(out=ac[:, :], in0=ZB[:, c, 0:T], scalar=CW[:, c, 0:1], in1=ac[:, :],
                                               op0=ALU.mult, op1=ALU.add)
                    S.dve.tensor_tensor(out=OC[:, c, :], in0=ac[:, :], in1=p_bg[:, :], op=ALU.mult)
                    S.pool.tensor_copy(out=ZB[:, c, 0:2], in_=ZB[:, c, T:T + 2])

                prev = None
                for c in range(4):
                    cur = pool_head(c)
                    conv_chunk(c)
                    if prev is not None:
                        pool_tail(*prev)
                    prev = (c, cur)
                pool_tail(*prev)
                if it + 1 < NT:
                    self.rmsnorm_T(XTs[(it + 1) % 2], HTs[(it + 1) % 2], SQ, RS, L, T)
                for dc in range(8):
                    pb = self.bank()
                    for kc in range(8):
                        S.pe.matmul(out=pb[:, :], lhsT=WO[:, kc, dc * 128:(dc + 1) * 128], rhs=OC[:, kc, :],
                                    start=(kc == 0), stop=(kc == 7))
                    S.dve.tensor_tensor(out=XT[:, dc, :], in0=XT[:, dc, :], in1=pb[:, :], op=ALU.add)
                    self.store_x_chunk(XT, dst, t0, T, dc)
            S.barrier()

    def even_phase(self, L, src, dst, pid):
        S = self.S
        nc = self.nc
        j = L // 2
        T = 256
        NT = self.seq // T
        NKT = self.seq // 128
        PB = self.PB
        PH = self.PH
        C_QA, C_KA, C_VA, C_RA, C_GL, C_QB, C_KB, C_VB = 0, 256, 512, 1024, 1536, 1552, 2064, 2576
        with ExitStack() as es:
            def sb(name, shape, dt):
                return es.enter_context(nc.sbuf_tensor(f"{name}_{pid}", list(shape), dt))
            WI = sb("wi", [128, 8, 3088], BF16)
            WO = sb("wo", [128, 8, D], BF16)
            KT = [sb(f"kt{h}", [128, self.seq], BF16) for h in range(4)]
            VA = sb("vaug", [128, NKT, 8, 65], BF16)
            XTs = [sb(f"xt{i}", [128, 8, T], F32) for i in range(2)]
            HT = sb("ht", [128, 8, T], BF16)
            OC = sb("oc", [128, 8, T], BF16)
            SQ = [sb(f"sq{i}", [128, T], BF16) for i in range(2)]
            RS = sb("rs", [128, T], F32)
            RS2 = [sb("rs20", [128, T], F32)] * 2
            SQ8 = sb("sq8", [128, 8, T], BF16)
            IDB = sb("idb", [128, 128], BF16)
            TRU = sb("tru", [128, 128], BF16)
            TRL = sb("trl", [128, 128], BF16)
            CM = sb("cm", [128, 512], BF16)
            BO = sb("bo", [128, 128], BF16)
            OH = sb("oh", [128, 16, 128], BF16)
            OWN = sb("own", [128, 8, 2, 256], BF16)
            ALB = sb("alb", [128, 8, 32], F32)
            ALQ = sb("alq", [128, 2, 8], F32)
            WG = sb("wg", [17, 256], BF16)
            GG = sb("gg", [128, 1], F32)
            QN = sb("qn", [128, 1], F32)
            KN = sb("kn", [128, 1], F32)
            GQK = sb("gqk", [128, 1], F32)
            QAT = sb("qat", [128, 2, T], BF16)
            KAT = sb("kat", [128, 2, T], BF16)
            GLR = sb("glr", [17, T], BF16)
            G_ = sb("g", [128, 256], F32)
            GH = sb("gh", [128, 256], BF16)
            GL_ = sb("gl", [128, 256], BF16)
            EB = sb("eb", [128, 128], F32)
            ENB = sb("enb", [128, 128], F32)
            DEC = sb("dec", [128, 2], F32)
            QS = sb("qs", [128, 4, 128], BF16)
            KS = sb("ks", [128, 2, 128], BF16)
            ATM = sb("atm", [128, 512], BF16)
            EXE = sb("exe", [128, 256], F32)
            KEND = sb("kend", [128, 256], BF16)
            KATK = sb("katk", [128, 256], F32)
            VTOK = sb("vtok", [128, 512], BF16)
            SIL = sb("sil", [128, 512], F32)
            OAT = sb("oat", [128, 512], BF16)
            ST = sb("st", [128, 2, 128], F32)
            STB = sb("stb", [128, 2, 256], BF16)
            SSQ = sb("ssq", [128, 4], F32)
            QT = [sb(f"qt{h}", [128, T], BF16) for h in range(8)]
            KM = [sb(f"km{h}", [128, 16], BF16) for h in range(4)]
            KMF = sb("kmf", [128, 1], F32)
            GSB = sb("gsb", [128, 2, 8, 16], F32)
            M3 = sb("m3", [128, 16], F32)
            GT = [sb(f"gt{i}", [128, 16, 16], F32) for i in range(2)]
            MB = sb("mb", [128, 2, 8, 16], BF16)
            MBT = sb("mbt", [128, 8, T], BF16)
            PT = [sb(f"pt{i}", [128, T], BF16) for i in range(3)]
            RD = sb("rd", [128, 2], F32)
            OBT = sb("obt", [128, 2, 512], BF16)

            self.load_x(XTs[0], src, 0, T)
            wi = self.dr["even_w_in"]
            wo = self.dr["w_o"]
            for kc in range(8):
                S.pool.dma_start(out=WI[:, kc, :], in_=wi[j, kc * 128:(kc + 1) * 128, :])
            S.pool.dma_start(out=WO[:, :, :], in_=wo[L].rearrange("(a p) n -> p a n", p=128))
            S.pool.dma_start(out=IDB[:], in_=self.dr["ident"])
            S.pool.dma_start(out=TRU[:], in_=self.dr["triu"])
            S.pool.dma_start(out=TRL[:], in_=self.dr["tril"])
            S.pool.dma_start(out=CM[:], in_=self.dr["cmask"])
            S.pool.dma_start(out=BO[:], in_=self.dr["blockones"])
            S.pool.dma_start(out=OH[:], in_=self.dr["onehot"])
            S.pool.dma_start(out=OWN[:], in_=self.dr["ownmask"])
            S.sp.dma_start(out=ALB[:], in_=self.dr["alibib"])
            S.sp.dma_start(out=ALQ[:], in_=self.dr["alibiq"])
            S.pool.dma_start(out=WG[:], in_=self.dr["wg_aug"][j])
            S.sp.dma_start(out=GG[:], in_=self.dr["gla_gain"][j])
            S.sp.dma_start(out=QN[:], in_=self.dr["qn"][j])
            S.sp.dma_start(out=KN[:], in_=self.dr["kn"][j])
            S.dve.scalar_tensor_tensor(out=GQK[:], in0=QN[:], scalar=0.125, in1=KN[:], op0=ALU.mult, op1=ALU.mult)
            S.dve.memset(ap=GLR[:, :], constant=1.0)
            S.dve.memset(ap=ST[:, :, :], constant=0.0)
            S.dve.memset(ap=STB[:, :, :], constant=0.0)
            S.dve.memset(ap=QS[:, :, :], constant=0.0)
            S.dve.memset(ap=MBT[:, :, :], constant=0.0)
            for h in range(8):
                S.dve.memset(ap=QT[h][:, :], constant=0.0)
            S.dve.memset(ap=VA[:, :, :, :].rearrange("p k h c -> p (k h c)"), constant=1.0)
            for h in range(4):
                S.dve.memset(ap=KM[h][:, :], constant=0.0)

            def proj_f(col0, M):
                pb = self.bank(0, 4)
                for kc in range(8):
                    S.pe.matmul(out=pb[0:M, 0:T], lhsT=WI[:, kc, col0:col0 + M], rhs=HT[:, kc, 0:T],
                                start=(kc == 0), stop=(kc == 7))
                return pb

            def proj_t(ts, col0, N):
                pb = self.bank(0, 4)
                for kc in range(8):
                    S.pe.matmul(out=pb[:, 0:N], lhsT=HT[:, kc, ts * 128:(ts + 1) * 128], rhs=WI[:, kc, col0:col0 + N],
                                start=(kc == 0), stop=(kc == 7))
                return pb

            for it in range(NT):
                t0 = it * T
                b = it
                XT = XTs[it % 2]
                if it + 1 < NT:
                    self.load_x(XTs[(it + 1) % 2], src, t0 + T, T)
                if it == 0:
                    self.rmsnorm_T(XT, HT, SQ, RS, L, T)
                def qk_finish(hp, which, pb, sq):
                    p2 = self.bank(4, 7)
                    S.pe.matmul(out=p2[:, 0:T], lhsT=BO[:], rhs=sq[:, 0:T], start=True, stop=True)
                    r2 = RS2[hp % 2]
                    S.act.activation(out=r2[:, :], in_=p2[:, 0:T], func=AF.Ln, bias=self.epsc[:], scale=1.0 / 64)
                    S.act.activation(out=r2[:, :], in_=r2[:, :], func=AF.Exp, scale=-0.5)
                    if which == 0:
                        for e in range(2):
                            r0 = e * 64
                            S.dve.scalar_tensor_tensor(out=QT[2 * hp + e][r0:r0 + 64, :], in0=pb[r0:r0 + 64, 0:T],
                                                       scalar=GQK[r0:r0 + 64, 0:1], in1=r2[r0:r0 + 64, :],
                                                       op0=ALU.mult, op1=ALU.mult)
                    else:
                        S.dve.tensor_tensor(out=KT[hp][:, t0:t0 + T], in0=pb[:, 0:T], in1=r2[:, :], op=ALU.mult)
                        S.dve.tensor_reduce(out=KMF[:, :], in_=KT[hp][:, t0:t0 + T], axis=AX.X, op=ALU.add)
                        S.dve.tensor_copy(out=KM[hp][:, b:b + 1], in_=KMF[:, :])
                def qk_items(which):
                    prevqk = None
                    for hp in range(4):
                        pb = proj_f((C_QB if which == 0 else C_KB) + hp * 128, 128)
                        sq = SQ[hp % 2]
                        S.act.activation(out=sq[:, 0:T], in_=pb[:, 0:T], func=AF.Square)
                        if prevqk is not None:
                            qk_finish(*prevqk)
                        prevqk = (hp, which, pb, sq)
                    qk_finish(*prevqk)

                def kv_items():
                    qk_items(1)
                    for ts in range(2):
                        pb = proj_t(ts, C_VB, 512)
                        if ts == 0:
                            S.act.copy(out=VA[:, it * 2 + ts, :, 0:64], in_=pb[:, :].rearrange("p (h c) -> p h c", c=64))
                        else:
                            S.dve.tensor_copy(out=VA[:, it * 2 + ts, :, 0:64], in_=pb[:, :].rearrange("p (h c) -> p h c", c=64))

                qk_items(0)
                if b >= 1:
                    pg = self.bank(4, 7)
                    for ts in range(2):
                        for h in range(8):
                            hp, r0 = h // 2, (h % 2) * 64
                            c0 = (ts * 8 + h) * 16
                            S.pe.matmul(out=pg[:, c0:c0 + 16], lhsT=QT[h][:, ts * 128:(ts + 1) * 128],
                                        rhs=KM[hp][:, :], start=True, stop=True)
                    S.dve.memset(ap=GSB[:, :, :, :], constant=-1e30)
                    S.dve.tensor_copy(out=GSB[:, :, :, 0:b],
                                      in_=pg[:, 0:256].rearrange("p (t h n) -> p t h n", t=2, h=8)[:, :, :, 0:b])
                    G3 = GSB[:, :, :, :].rearrange("p t h n -> p (t h) n")
                    cur = G3
                    for r in range(3):
                        S.dve.tensor_reduce(out=M3[:, :], in_=cur, axis=AX.X, op=ALU.max)
                        if r == 2:
                            break
                        nxt = GT[r]
                        S.dve.tensor_tensor(out=nxt[:, :, :], in0=cur, in1=M3[:, :].unsqueeze(2).to_broadcast([128, 16, 16]),
                                            op=ALU.is_ge)
                        S.dve.scalar_tensor_tensor(out=nxt[:, :, :], in0=nxt[:, :, :], scalar=-2e30, in1=cur,
                                                   op0=ALU.mult, op1=ALU.add)
                        cur = nxt[:, :, :]
                    S.dve.tensor_tensor(out=GT[0][:, :, :], in0=G3, in1=M3[:, :].unsqueeze(2).to_broadcast([128, 16, 16]),
                                        op=ALU.is_lt)
                    S.dve.tensor_scalar(out=MB[:, :, :, :].rearrange("p t h n -> p (t h) n"), in0=GT[0][:, :, :],
                                        scalar1=NEG, scalar2=None, op0=ALU.mult)
                    S.dve.tensor_copy(out=MB[:, :, :, 15:16], in_=ALQ[:, :, :].rearrange("p t (h o) -> p t h o", o=1))
                kv_items()
                if b >= 1:
                    for ts in range(2):
                        for h in range(8):
                            S.pe.transpose(out=PH[0:16, h * 128:(h + 1) * 128], in_=MB[:, ts, h, :], identity=IDB[:])
                        S.act.copy(out=MBT[0:16, :, ts * 128:(ts + 1) * 128],
                                   in_=PH[0:16, :].rearrange("p (h q) -> p h q", h=8))
                def gla_gen():
                    gcnt = [0]

                    def gbank():
                        bk = PB[5 + gcnt[0] % 2]
                        gcnt[0] += 1
                        return bk

                    def gproj_f(col0, M):
                        pb = gbank()
                        for kc in range(8):
                            S.pe.matmul(out=pb[0:M, 0:T], lhsT=WI[:, kc, col0:col0 + M], rhs=HT[:, kc, 0:T],
                                        start=(kc == 0), stop=(kc == 7))
                        return pb

                    def gproj_t(ts, col0, N):
                        pb = gbank()
                        for kc in range(8):
                            S.pe.matmul(out=pb[:, 0:N], lhsT=HT[:, kc, ts * 128:(ts + 1) * 128], rhs=WI[:, kc, col0:col0 + N],
                                        start=(kc == 0), stop=(kc == 7))
                        return pb

                    for hp in range(2):
                        pb = gproj_f(C_QA + hp * 128, 128)
                        S.dve.tensor_copy(out=QAT[:, hp, :], in_=pb[:, 0:T])
                        yield
                        pb = gproj_f(C_KA + hp * 128, 128)
                        S.dve.tensor_copy(out=KAT[:, hp, :], in_=pb[:, 0:T])
                        yield
                    pb = gproj_f(C_GL, 16)
                    S.dve.tensor_copy(out=GLR[0:16, :], in_=pb[0:16, 0:T])
                    yield
                    for ts in range(2):
                        tc0 = ts * 128
                        pb = gproj_t(ts, C_VA, 512)
                        S.dve.tensor_copy(out=VTOK[:, :], in_=pb[:, :])
                        yield
                        pb = gproj_t(ts, C_RA, 512)
                        S.act.activation(out=SIL[:, :], in_=pb[:, :], func=AF.Exp, scale=-1.0)
                        yield
                        S.act.activation(out=SIL[:, :], in_=SIL[:, :], func=AF.Ln, bias=1.0, scale=1.0)
                        S.act.activation(out=SIL[:, :], in_=SIL[:, :], func=AF.Exp, scale=-1.0)
                        S.dve.tensor_tensor(out=SIL[:, :], in0=SIL[:, :], in1=pb[:, :], op=ALU.mult)
                        yield
                        pb = gproj_t(ts, C_KA, 256)
                        S.dve.tensor_copy(out=KATK[:, :], in_=pb[:, 0:256])
                        yield
                        pz = gbank()
                        S.pe.matmul(out=pz[:, 0:256], lhsT=GLR[:, tc0:tc0 + 128], rhs=WG[:, :], start=True, stop=True)
                        S.act.activation(out=G_[:, :], in_=pz[:, 0:256], func=AF.Exp, scale=-1.0)
                        S.act.activation(out=G_[:, :], in_=G_[:, :], func=AF.Ln, bias=1.0, scale=1.0)
                        S.dve.tensor_copy(out=GH[:, :], in_=G_[:, :])
                        S.dve.tensor_tensor(out=GL_[:, :], in0=G_[:, :], in1=GH[:, :], op=ALU.subtract)
                        yield
                        pe_ = gbank()
                        S.pe.matmul(out=pe_[:, 0:256], lhsT=TRL[:, :], rhs=GH[:, :], start=True, stop=False)
                        S.pe.matmul(out=pe_[:, 0:256], lhsT=TRL[:, :], rhs=GL_[:, :], start=False, stop=True)
                        S.act.activation(out=EXE[:, :], in_=pe_[:, 0:256], func=AF.Exp)
                        S.dve.tensor_tensor(out=KEND[:, :], in0=KATK[:, :], in1=EXE[:, :], op=ALU.mult)
                        yield
                        for hp in range(2):
                            pbt = gbank()
                            S.pe.matmul(out=pbt[:, 0:128], lhsT=GH[:, hp * 128:(hp + 1) * 128], rhs=TRU[:, :],
                                        start=True, stop=False)
                            S.pe.matmul(out=pbt[:, 0:128], lhsT=GL_[:, hp * 128:(hp + 1) * 128], rhs=TRU[:, :],
                                        start=False, stop=True)
                            S.act.activation(out=EB[:, :], in_=pbt[:, 0:128], func=AF.Exp)
                            S.act.activation(out=ENB[:, :], in_=pbt[:, 0:128], func=AF.Exp, scale=-1.0)
                            yield
                            S.dve.tensor_copy(out=DEC[:, hp:hp + 1], in_=EB[:, 127:128])
                            for e in range(2):
                                r0 = e * 64
                                S.dve.scalar_tensor_tensor(out=QS[r0:r0 + 64, 2 * hp + e, :], in0=QAT[r0:r0 + 64, hp, tc0:tc0 + 128],
                                                           scalar=0.125, in1=EB[r0:r0 + 64, :], op0=ALU.mult, op1=ALU.mult)
                            S.dve.tensor_tensor(out=KS[:, hp, :], in0=KAT[:, hp, tc0:tc0 + 128], in1=ENB[:, :], op=ALU.mult)
                            yield
                        for hp in range(2):
                            pds = gbank()
                            S.pe.matmul(out=pds[:, 0:256], lhsT=KEND[:, hp * 128:(hp + 1) * 128],
                                        rhs=VTOK[:, hp * 256:(hp + 1) * 256], start=True, stop=True)
                            for e in range(2):
                                r0 = e * 64
                                S.dve.scalar_tensor_tensor(out=ST[r0:r0 + 64, hp, :], in0=ST[r0:r0 + 64, hp, :],
                                                           scalar=DEC[r0:r0 + 64, hp:hp + 1],
                                                           in1=pds[r0:r0 + 64, e * 128:(e + 1) * 128],
                                                           op0=ALU.mult, op1=ALU.add)
                            yield
                        pat = gbank()
                        for h in range(4):
                            S.pe.matmul(out=pat[:, h * 128:(h + 1) * 128], lhsT=KS[:, h // 2, :],
                                        rhs=QS[:, h, :], start=True, stop=True)
                        S.dve.tensor_tensor(out=ATM[:, :], in0=pat[:, :], in1=CM[:, :], op=ALU.mult)
                        yield
                        po = gbank()
                        for h in range(4):
                            hp = h // 2
                            S.pe.matmul(out=po[:, h * 128:(h + 1) * 128], lhsT=ATM[:, h * 128:(h + 1) * 128],
                                        rhs=VTOK[:, h * 128:(h + 1) * 128], start=(h == 0), stop=False,
                                        skip_group_check=True)
                            S.pe.matmul(out=po[:, hp * 256:(hp + 1) * 256], lhsT=QS[:, h, :],
                                        rhs=STB[:, hp, :], start=False, stop=True, skip_group_check=True)
                        yield
                        for hp in range(2):
                            for e in range(2):
                                r0 = e * 64
                                S.act.copy(out=STB[r0:r0 + 64, hp, e * 128:(e + 1) * 128], in_=ST[r0:r0 + 64, hp, :])
                        for h in range(4):
                            S.act.activation(out=OAT[:, h * 128:(h + 1) * 128], in_=po[:, h * 128:(h + 1) * 128], func=AF.Square,
                                             accum_out=SSQ[:, h:h + 1])
                        yield
                        S.act.activation(out=SSQ[:, :], in_=SSQ[:, :], func=AF.Ln, bias=self.epsc[:], scale=1.0 / 128)
                        S.act.activation(out=SSQ[:, :], in_=SSQ[:, :], func=AF.Exp, scale=-0.5)
                        for h in range(4):
                            S.dve.scalar_tensor_tensor(out=OAT[:, h * 128:(h + 1) * 128], in0=po[:, h * 128:(h + 1) * 128],
                                                       scalar=SSQ[:, h:h + 1], in1=SIL[:, h * 128:(h + 1) * 128],
                                                       op0=ALU.mult, op1=ALU.mult)
                        yield
                        for h in range(4):
                            S.pe.transpose(out=PH[:, h * 128:(h + 1) * 128], in_=OAT[:, h * 128:(h + 1) * 128], identity=IDB[:])
                        for h in range(4):
                            S.dve.tensor_scalar(out=OC[:, h, tc0:tc0 + 128], in0=PH[:, h * 128:(h + 1) * 128],
                                                scalar1=GG[:, 0:1], scalar2=None, op0=ALU.mult)
                        yield
                nkt = 2 * b + 2
                PAST_CAP = (1, 2, 4, 8, 16, 16, 16, 16)
                kt_lo = [max(0, 2 * (b - PAST_CAP[h])) for h in range(8)]
                items = [(h, kt) for h in range(8) for kt in range(kt_lo[h], nkt)]
                first_of = {}
                scnt = [0]

                def att_s1(h, kt):
                    hp = h // 2
                    ps = PB[scnt[0] % 3]
                    scnt[0] += 1
                    S.pe.matmul(out=ps[:, 0:T], lhsT=KT[hp][:, kt * 128:(kt + 1) * 128],
                                rhs=QT[h][:, :], start=True, stop=False)
                    if kt < 2 * b:
                        S.pe.matmul(out=ps[:, 0:T], lhsT=OH[:, kt // 2, :], rhs=MBT[:, h, :], start=False, stop=True)
                    else:
                        S.pe.matmul(out=ps[:, 0:T], lhsT=IDB[:, :], rhs=OWN[:, h, kt - 2 * b, :], start=False, stop=True)
                    return ps

                def att_s23(h, kt, ps, cnt):
                    pacc = PB[3 + (h % 2)]
                    pt = PT[cnt % 3]
                    if kt < 2 * b:
                        dl = 2 * b - kt
                        S.act.activation(out=pt[:, :], in_=ps[:, 0:T], func=AF.Exp, bias=ALB[:, h, dl:dl + 1], scale=1.0)
                    else:
                        S.act.activation(out=pt[:, :], in_=ps[:, 0:T], func=AF.Exp)
                    for ts in range(2):
                        if kt == 2 * b + 1 and ts == 0:
                            continue
                        S.pe.matmul(out=pacc[:, ts * 65:(ts + 1) * 65], lhsT=pt[:, ts * 128:(ts + 1) * 128],
                                    rhs=VA[:, kt, h, :], start=(h not in first_of), stop=(kt == nkt - 1),
                                    skip_group_check=True)
                        first_of[h] = True
                    if kt == nkt - 1:
                        S.dve.reciprocal(out=RD[:, :], in_=pacc[:, 0:130].rearrange("p (t c) -> p t c", c=65)[:, :, 64])
                        for ts in range(2):
                            S.dve.tensor_scalar(out=OBT[:, ts, h * 64:(h + 1) * 64], in0=pacc[:, ts * 65:ts * 65 + 64],
                                                scalar1=RD[:, ts:ts + 1], scalar2=None, op0=ALU.mult)

                if it + 1 < NT:
                    XN = XTs[(it + 1) % 2]
                    for c in range(8):
                        S.act.activation(out=SQ8[:, c, :], in_=XN[:, c, 0:T], func=AF.Square)
                gen = gla_gen()
                gen_done = [False]

                def gla_step(k):
                    for _ in range(k):
                        if gen_done[0]:
                            return
                        try:
                            next(gen)
                        except StopIteration:
                            gen_done[0] = True

                NUNITS = 50
                pend = []
                cnt = 0
                done_units = 0
                for ii, (h, kt) in enumerate(items):
                    pend.append((h, kt, att_s1(h, kt)))
                    if len(pend) > 2:
                        h0, k0, ps0 = pend.pop(0)
                        att_s23(h0, k0, ps0, cnt)
                        cnt += 1
                    want = -(-(ii + 1) * NUNITS // len(items))
                    gla_step(want - done_units)
                    done_units = want
                while pend:
                    h0, k0, ps0 = pend.pop(0)
                    att_s23(h0, k0, ps0, cnt)
                    cnt += 1
                gla_step(10 ** 6)
                if it + 1 < NT:
                    pn = PB[5]
                    for c in range(8):
                        S.pe.matmul(out=pn[:, 0:T], lhsT=self.ones_bf[:], rhs=SQ8[:, c, :], start=(c == 0), stop=(c == 7))
                    S.act.activation(out=RS[:, 0:T], in_=pn[:, 0:T], func=AF.Ln, bias=self.epsc[:], scale=1.0 / D)
                    S.act.activation(out=RS[:, 0:T], in_=RS[:, 0:T], func=AF.Exp, scale=-0.5)
                    for c in range(8):
                        S.dve.scalar_tensor_tensor(out=HT[:, c, 0:T], in0=XN[:, c, 0:T], scalar=self.gains[:, L, c:c + 1],
                                                   in1=RS[:, 0:T], op0=ALU.mult, op1=ALU.mult)
                for ts in range(2):
                    for hp in range(4):
                        S.pe.transpose(out=PH[:, hp * 128:(hp + 1) * 128], in_=OBT[:, ts, hp * 128:(hp + 1) * 128],
                                       identity=IDB[:])
                    S.dve.tensor_copy(out=OC[:, 4:8, ts * 128:(ts + 1) * 128],
                                      in_=PH[:, 0:512].rearrange("p (h q) -> p h q", h=4))
                for dc in range(8):
                    pb = self.bank(0, 5)
                    for kc in range(8):
                        S.pe.matmul(out=pb[:, 0:T], lhsT=WO[:, kc, dc * 128:(dc + 1) * 128], rhs=OC[:, kc, :],
                                    start=(kc == 0), stop=(kc == 7))
                    S.dve.tensor_tensor(out=XT[:, dc, :], in0=XT[:, dc, :], in1=pb[:, 0:T], op=ALU.add)
                    self.store_x_chunk(XT, dst, t0, T, dc)
            S.barrier()


FULL_PLAN = []
for _l in range(DEPTH):
    FULL_PLAN.append(("even" if _l % 2 == 0 else "odd", _l))
    FULL_PLAN.append(("mlp", _l))


def prep_params(inp):
    p = {}
    g = np.concatenate([np.asarray(inp["norm_mix"], np.float32), np.asarray(inp["norm_mlp"], np.float32)], axis=0)
    p["gains"] = np.ascontiguousarray(g.reshape(8, 8, 128).transpose(2, 0, 1))
    for k in ("w_o", "w1", "w2", "even_w_in", "odd_w_in"):
        p[k] = np.ascontiguousarray(np.asarray(inp[k], np.float32))
    p["wg_aug"] = np.ascontiguousarray(np.concatenate(
        [np.asarray(inp["gla_w_gate2"], np.float32), np.asarray(inp["gla_b_gate"], np.float32)[:, None, :]], axis=1))
    p["gla_gain"] = np.ascontiguousarray(np.asarray(inp["gla_out_norm"], np.float32)[:, :, None])
    p["qn"] = np.ascontiguousarray(np.tile(np.asarray(inp["moba_q_norm"], np.float32), (1, 2))[:, :, None])
    p["kn"] = np.ascontiguousarray(np.tile(np.asarray(inp["moba_k_norm"], np.float32), (1, 2))[:, :, None])
    cw = np.asarray(inp["conv_w"], np.float32)
    p["convw"] = np.ascontiguousarray(cw.reshape(2, 3, 4, 128).transpose(3, 0, 2, 1))
    ps = np.asarray(inp["pool_scale"], np.float32)
    p["poolscale"] = np.ascontiguousarray(ps.reshape(2, 4, 128).transpose(2, 0, 1))
    pw = np.asarray(inp["pool_w"], np.float32)
    p["poolw"] = np.ascontiguousarray(pw.transpose(0, 2, 1, 3))
    return p


_CACHE = {}


def run_plan(plan, inputs, x_list, seq=SEQ):
    key = (tuple(plan), seq)
    if key not in _CACHE:
        _CACHE[key] = Builder(plan, seq)
    bld = _CACHE[key]
    base = dict(host_constants())
    base.update(prep_params(inputs))
    in_maps = []
    for xb in x_list:
        m = dict(base)
        m["xT"] = np.ascontiguousarray(np.asarray(xb, np.float32).T)
        in_maps.append(m)
    res = run_bass_kernel_spmd(bld.nc, in_maps, core_ids=list(range(len(x_list))))
    return [np.ascontiguousarray(r["yT"].T) for r in res.results]


def kernel(**inputs):
    x = np.asarray(inputs["x"], np.float32)
    outs = run_plan(FULL_PLAN, inputs, [x[b] for b in range(x.shape[0])])
    return np.stack(outs, axis=0).astype(np.float32)
```

```python
from contextlib import ExitStack

import numpy as np
import concourse.bass as bass
import concourse.mybir as mybir
from concourse.bass_utils import run_bass_kernel_spmd

F32 = mybir.dt.float32
BF16 = mybir.dt.bfloat16
AF = mybir.ActivationFunctionType
ALU = mybir.AluOpType
AX = mybir.AxisListType

D = 1024
SEQ = 4096
DEPTH = 4
DFF = 4096
EPS = 1e-6
NEG = -30000.0


class _Op:
    __slots__ = ("eng", "fn", "deps", "dma", "signal", "seq", "sem", "idx")


class _EngProxy:
    def __init__(self, sched, name):
        self._s = sched
        self._n = name

    def __getattr__(self, meth):
        def call(**kw):
            return self._s._record(self._n, meth, kw)
        return call


WRITE_KEYS = ("out", "accum_out", "ap")
EPOCH = 12000
NDMASEM = 40
NSW = 20


class Sched:
    def __init__(self, nc):
        self.nc = nc
        self.engs = {"pe": nc.tensor, "act": nc.scalar, "dve": nc.vector,
                     "pool": nc.gpsimd, "sp": nc.sync}
        self.pe = _EngProxy(self, "pe")
        self.act = _EngProxy(self, "act")
        self.dve = _EngProxy(self, "dve")
        self.pool = _EngProxy(self, "pool")
        self.sp = _EngProxy(self, "sp")
        self.ops = []
        self.emitted = 0
        self.recs = {}
        self.untracked = set()
        self._stack = []
        self.cnt = {e: 0 for e in self.engs}
        self.engsems = {}
        self.dmasems = None
        self.dmacnt = [0] * NDMASEM
        self.dmalast = [None] * NDMASEM
        self.ndma = 0
        self.nsw = 0
        self.waited = {e: {} for e in self.engs}
        self.last_op = {}
        self.open_dmas = []
        self.barrier_deps = None
        self.barrier_done = set()

    def keep(self, cm):
        t = cm.__enter__()
        self._stack.append(cm)
        return t

    def _sem(self, name):
        return self.keep(self.nc.semaphore(name))

    @staticmethod
    def _box(ap):
        if ap.tensor.name.startswith("pb"):
            return (0, 128, 0, 1 << 30)
        shp = ap.tensor.shape
        pstride = 1
        for d in shp[1:]:
            pstride *= int(d)
        off = int(ap.offset)
        p0 = off // pstride
        f0 = off % pstride
        pe = 0
        fe = 0
        for step, cnt in ap.ap:
            step = int(step)
            cnt = int(cnt)
            if cnt <= 1:
                continue
            if step >= pstride and step % pstride == 0:
                pe += (cnt - 1) * (step // pstride)
            else:
                fe += (cnt - 1) * step
        return (p0, p0 + pe + 1, f0, f0 + fe + 1)

    def _record(self, eng, meth, kw):
        idx = len(self.ops)
        op = _Op()
        op.eng = eng
        op.idx = idx
        op.dma = meth in ("dma_start", "dma_start_transpose")
        op.signal = False
        op.seq = None
        op.sem = None
        deps = set()
        if self.barrier_deps is not None and eng not in self.barrier_done:
            deps.update(self.barrier_deps)
            self.barrier_done.add(eng)
        engkey = ("dma", idx) if op.dma else eng
        reads = []
        writes = []
        for k, v in kw.items():
            if isinstance(v, bass.AP):
                (writes if k in WRITE_KEYS else reads).append(v)
        for ap in reads:
            nm = ap.tensor.name
            if nm in self.untracked:
                continue
            b = self._box(ap)
            lst = self.recs.get(nm, ())
            new = []
            for r in lst:
                ov = not (r[1] <= b[0] or b[1] <= r[0] or r[3] <= b[2] or b[3] <= r[2])
                if ov and r[5]:
                    deps.add(r[4])
                if (not r[5]) and r[6] == engkey and b[0] <= r[0] and r[1] <= b[1] and b[2] <= r[2] and r[3] <= b[3]:
                    continue
                new.append(r)
            new.append((b[0], b[1], b[2], b[3], idx, False, engkey))
            self.recs[nm] = new
        for ap in writes:
            nm = ap.tensor.name
            b = self._box(ap)
            lst = self.recs.get(nm, ())
            new = []
            for r in lst:
                ov = not (r[1] <= b[0] or b[1] <= r[0] or r[3] <= b[2] or b[3] <= r[2])
                if ov:
                    deps.add(r[4])
                    if b[0] <= r[0] and r[1] <= b[1] and b[2] <= r[2] and r[3] <= b[3]:
                        continue
                new.append(r)
            new.append((b[0], b[1], b[2], b[3], idx, True, engkey))
            self.recs[nm] = new
        deps.discard(idx)
        op.deps = deps
        op.fn = (meth, kw)
        self.ops.append(op)
        if op.dma:
            self.open_dmas.append(op)
        else:
            self.last_op[eng] = op
        return op

    def _do_wait(self, eng, sem, key, val):
        w = self.waited[eng]
        if w.get(key, 0) >= val:
            return
        w[key] = val
        self.engs[eng].wait_ge(sem, val)

    def flush(self):
        ops = self.ops
        if self.dmasems is None:
            self.dmasems = [self._sem(f"dq{i}") for i in range(NDMASEM)]
        pend = ops[self.emitted:]
        for op in pend:
            for j in op.deps:
                d = ops[j]
                if (not d.dma) and d.eng == "pe" and op.eng == "pe" and not op.dma:
                    continue
                d.signal = True
        for op in pend:
            eng = op.eng
            e = self.engs[eng]
            for j in sorted(op.deps):
                d = ops[j]
                if (not d.dma) and d.eng == "pe" and eng == "pe" and not op.dma:
                    continue
                self._do_wait(eng, d.sem[0], d.sem[1], d.seq)
            if op.dma:
                op.signal = True
                if eng == "pool":
                    si = self.nsw % NSW
                    self.nsw += 1
                else:
                    si = NSW + self.ndma % (NDMASEM - NSW)
                    self.ndma += 1
                prev = self.dmalast[si]
                if prev is not None:
                    self._do_wait(eng, prev.sem[0], prev.sem[1], prev.seq)
                self.dmacnt[si] += 16
                op.sem = (self.dmasems[si], ("d", si))
                op.seq = self.dmacnt[si]
                self.dmalast[si] = op
            elif op.signal:
                self.cnt[eng] += 1
                ep = self.cnt[eng] // EPOCH
                if self.cnt[eng] - ep * EPOCH == 0:
                    self.cnt[eng] += 1
                key = (eng, ep)
                if key not in self.engsems:
                    self.engsems[key] = self._sem(f"s_{eng}_{ep}")
                op.sem = (self.engsems[key], key)
                op.seq = self.cnt[eng] - ep * EPOCH
            meth, kw = op.fn
            ins = getattr(e, meth)(**kw)
            if op.signal:
                ins.then_inc(op.sem[0], 16 if op.dma else 1)
            op.fn = None
        self.emitted = len(ops)

    def barrier(self):
        deps = []
        for eng, op in self.last_op.items():
            op.signal = True
            deps.append(op.idx)
        self.flush()
        for si in range(NDMASEM):
            if self.dmalast[si] is not None:
                deps.append(self.dmalast[si].idx)
        self.barrier_deps = deps
        self.barrier_done = set()
        self.recs = {}
        self.open_dmas = []

    def finish(self):
        self.barrier()
        for j in self.barrier_deps:
            d = self.ops[j]
            self._do_wait("sp", d.sem[0], d.sem[1], d.seq)


def alibi_slopes():
    return [2.0 ** (-(h + 1)) for h in range(8)]


def host_constants():
    c = {}
    c["ident"] = np.eye(128, dtype=np.float32)
    c["onesm"] = np.ones((128, 128), np.float32)
    jj = np.arange(128)[:, None]
    ii = np.arange(128)[None, :]
    c["triu"] = np.where(jj <= ii, -1.0 / 16.0, 0.0).astype(np.float32)
    c["tril"] = np.where(jj > ii, -1.0 / 16.0, 0.0).astype(np.float32)
    c["cmask"] = np.tile(np.where(jj <= ii, 1.0, 0.0).astype(np.float32), (1, 4))
    c["blockones"] = np.kron(np.eye(2, dtype=np.float32), np.ones((64, 64), np.float32))
    oh = np.zeros((128, 16, 128), np.float32)
    for n in range(16):
        oh[n, n, :] = 1.0
        oh[15, n, :] = 1.0
    c["onehot"] = oh
    sl = alibi_slopes()
    own = np.zeros((128, 8, 2, 256), np.float32)
    kl = np.arange(128)[:, None]
    ql = np.arange(256)[None, :]
    for h in range(8):
        for j in range(2):
            pk = j * 128 + kl
            dist = (ql - pk).astype(np.float32)
            own[:, h, j, :] = np.where(pk <= ql, -sl[h] * dist, NEG)
    c["ownmask"] = own
    ab = np.zeros((128, 8, 32), np.float32)
    for h in range(8):
        for dl in range(32):
            ab[:, h, dl] = sl[h] * (np.arange(128) - dl * 128.0)
    c["alibib"] = ab
    aq = np.zeros((128, 2, 8), np.float32)
    for ts in range(2):
        for h in range(8):
            aq[:, ts, h] = -sl[h] * (ts * 128 + np.arange(128))
    c["alibiq"] = aq
    return c


CONST_SHAPES = {
    "ident": [128, 128], "onesm": [128, 128], "triu": [128, 128], "tril": [128, 128],
    "cmask": [128, 512], "blockones": [128, 128], "onehot": [128, 16, 128],
    "ownmask": [128, 8, 2, 256], "alibib": [128, 8, 32], "alibiq": [128, 2, 8],
}

PARAM_SHAPES = {
    "gains": [128, 8, 8],
    "w_o": [4, 1024, 1024], "w1": [4, 1024, 4096], "w2": [4, 4096, 1024],
    "even_w_in": [2, 1024, 3088], "odd_w_in": [2, 1024, 2048],
    "wg_aug": [2, 17, 256], "gla_gain": [2, 128, 1], "qn": [2, 128, 1], "kn": [2, 128, 1],
    "convw": [128, 2, 4, 3], "poolscale": [128, 2, 4], "poolw": [2, 128, 4, 128],
}


class Builder:
    def __init__(self, plan, seq=SEQ):
        self.plan = plan
        self.seq = seq
        nc = bass.Bass("TRN2", target_bir_lowering=False)
        self.nc = nc
        self.S = Sched(nc)
        S = self.S
        self.dr = {}
        for nm, shp in list(CONST_SHAPES.items()) + list(PARAM_SHAPES.items()):
            self.dr[nm] = nc.dram_tensor(nm, shp, F32, kind="ExternalInput").ap()
            S.untracked.add(nm)
        self.xT = nc.dram_tensor("xT", [D, seq], F32, kind="ExternalInput").ap()
        S.untracked.add("xT")
        self.yT = nc.dram_tensor("yT", [D, seq], F32, kind="ExternalOutput").ap()
        self.xs = nc.dram_tensor("xs", [D, seq], F32, kind="Internal").ap()
        self.PB = [S.keep(nc.psum_tensor(f"pb{i}", [128, 512], F32)) for i in range(7)]
        self.PH = S.keep(nc.psum_tensor("pbh", [128, 1024], BF16))
        self.ones_bf = S.keep(nc.sbuf_tensor("ones_bf", [128, 128], BF16))
        self.gains = S.keep(nc.sbuf_tensor("gains_sb", [128, 8, 8], F32))
        self.epsc = S.keep(nc.sbuf_tensor("epsc", [128, 1], F32))
        S.pool.dma_start(out=self.ones_bf[:], in_=self.dr["onesm"])
        S.sp.dma_start(out=self.gains[:], in_=self.dr["gains"])
        S.dve.memset(ap=self.epsc[:], constant=EPS)
        self._bank = 0
        n = len(plan)
        for i, (kind, L) in enumerate(plan):
            src = self.xT if i == 0 else self.xs
            dst = self.yT if i == n - 1 else self.xs
            if kind == "mlp":
                self.mlp_phase(L, src, dst, i)
            elif kind == "odd":
                self.odd_phase(L, src, dst, i)
            elif kind == "even":
                self.even_phase(L, src, dst, i)
            else:
                raise ValueError(kind)
        S.finish()

    def bank(self, lo=0, hi=7):
        b = self.PB[lo + self._bank % (hi - lo)]
        self._bank += 1
        return b

    def load_x(self, XT, src, t0, T):
        S = self.S
        v = src[:, t0:t0 + T].rearrange("(c p) t -> p c t", p=128)
        for c in range(8):
            S.sp.dma_start(out=XT[:, c, 0:T], in_=v[:, c, :])

    def load_x_chunk(self, XT, src, t0, T, c):
        v = src[:, t0:t0 + T].rearrange("(c p) t -> p c t", p=128)
        self.S.sp.dma_start(out=XT[:, c, 0:T], in_=v[:, c, :])

    def store_x_chunk(self, XT, dst, t0, T, c):
        v = dst[:, t0:t0 + T].rearrange("(c p) t -> p c t", p=128)
        self.S.sp.dma_start(out=v[:, c, :], in_=XT[:, c, 0:T])

    def rmsnorm_T(self, XT, HT, SQ, RS, gi, T):
        S = self.S
        pb = self.bank()
        for c in range(8):
            sq = SQ[c % 2]
            S.act.activation(out=sq[:, 0:T], in_=XT[:, c, 0:T], func=AF.Square)
            S.pe.matmul(out=pb[:, 0:T], lhsT=self.ones_bf[:], rhs=sq[:, 0:T], start=(c == 0), stop=(c == 7))
        S.act.activation(out=RS[:, 0:T], in_=pb[:, 0:T], func=AF.Ln, bias=self.epsc[:], scale=1.0 / D)
        S.act.activation(out=RS[:, 0:T], in_=RS[:, 0:T], func=AF.Exp, scale=-0.5)
        for c in range(8):
            S.dve.scalar_tensor_tensor(out=HT[:, c, 0:T], in0=XT[:, c, 0:T], scalar=self.gains[:, gi, c:c + 1],
                                       in1=RS[:, 0:T], op0=ALU.mult, op1=ALU.mult)

    def mlp_phase(self, L, src, dst, pid):
        S = self.S
        nc = self.nc
        T = 512
        NT = self.seq // T
        with ExitStack() as es:
            def sb(name, shape, dt):
                return es.enter_context(nc.sbuf_tensor(f"{name}_{pid}", list(shape), dt))
            W1 = sb("w1s", [128, 8, DFF], BF16)
            W2 = sb("w2s", [128, 32, D], BF16)
            XTs = [sb(f"xt{i}", [128, 8, T], F32) for i in range(2)]
            HTs = [sb(f"ht{i}", [128, 8, T], BF16) for i in range(2)]
            AT = sb("at", [128, 16, T], BF16)
            SQ8 = sb("sq8", [128, 8, T], BF16)
            SQ = [sb(f"sq{i}", [128, T], BF16) for i in range(2)]
            RS = sb("rs", [128, T], F32)
            RL = [sb(f"rl{i}", [128, T], BF16) for i in range(2)]
            w1 = self.dr["w1"]
            w2 = self.dr["w2"]
            self.load_x(XTs[0], src, 0, T)
            for kc in range(8):
                S.pool.dma_start(out=W1[:, kc, :], in_=w1[L, kc * 128:(kc + 1) * 128, :])
            for f4 in range(8):
                S.pool.dma_start(out=W2[:, f4 * 4:(f4 + 1) * 4, :],
                                 in_=w2[L, f4 * 512:(f4 + 1) * 512, :].rearrange("(a p) n -> p a n", p=128))
            self.rmsnorm_T(XTs[0], HTs[0], SQ, RS, 4 + L, T)
            for it in range(NT):
                t0 = it * T
                XT = XTs[it % 2]
                HT = HTs[it % 2]
                nxt = it + 1 < NT
                if nxt:
                    XN = XTs[(it + 1) % 2]
                    HN = HTs[(it + 1) % 2]
                    self.load_x(XN, src, t0 + T, T)
                for half in range(2):
                    for f in range(16):
                        fc = half * 16 + f
                        pb = self.bank()
                        for kc in range(8):
                            S.pe.matmul(out=pb[:, :], lhsT=W1[:, kc, fc * 128:(fc + 1) * 128], rhs=HT[:, kc, :],
                                        start=(kc == 0), stop=(kc == 7))
                        rl = RL[fc % 2]
                        S.act.activation(out=rl[:, :], in_=pb[:, :], func=AF.Relu)
                        S.dve.tensor_tensor(out=AT[:, f, :], in0=rl[:, :], in1=rl[:, :], op=ALU.mult)
                        if nxt and half == 0 and f == 2:
                            for c in range(8):
                                S.act.activation(out=SQ8[:, c, :], in_=XN[:, c, :], func=AF.Square)
                        if nxt and half == 1 and f == 2:
                            pn = self.bank()
                            for c in range(8):
                                S.pe.matmul(out=pn[:, :], lhsT=self.ones_bf[:], rhs=SQ8[:, c, :], start=(c == 0), stop=(c == 7))
                            S.act.activation(out=RS[:, :], in_=pn[:, :], func=AF.Ln, bias=self.epsc[:], scale=1.0 / D)
                            S.act.activation(out=RS[:, :], in_=RS[:, :], func=AF.Exp, scale=-0.5)
                            for c in range(8):
                                S.dve.scalar_tensor_tensor(out=HN[:, c, :], in0=XN[:, c, :], scalar=self.gains[:, 4 + L, c:c + 1],
                                                           in1=RS[:, :], op0=ALU.mult, op1=ALU.mult)
                    for dc in range(8):
                        pb = self.bank()
                        for f in range(16):
                            fc = half * 16 + f
                            S.pe.matmul(out=pb[:, :], lhsT=W2[:, fc, dc * 128:(dc + 1) * 128], rhs=AT[:, f, :],
                                        start=(f == 0), stop=(f == 15))
                        S.dve.tensor_tensor(out=XT[:, dc, :], in0=XT[:, dc, :], in1=pb[:, :], op=ALU.add)
                        if half == 1:
                            self.store_x_chunk(XT, dst, t0, T, dc)
            S.barrier()

    def odd_phase(self, L, src, dst, pid):
        S = self.S
        nc = self.nc
        j = L // 2
        T = 512
        NT = self.seq // T
        WIN = (2, 4, 8, 16)
        W_ = T + 16
        with ExitStack() as es:
            def sb(name, shape, dt):
                return es.enter_context(nc.sbuf_tensor(f"{name}_{pid}", list(shape), dt))
            WI = sb("wi", [128, 8, 2048], BF16)
            WO = sb("wo", [128, 8, D], BF16)
            PW = sb("pw", [128, 4, 128], BF16)
            CW = sb("cw", [128, 4, 3], F32)
            PSC = sb("psc", [128, 4], F32)
            XTs = [sb(f"xt{i}", [128, 8, T], F32) for i in range(2)]
            HTs = [sb(f"ht{i}", [128, 8, T], BF16) for i in range(2)]
            OC = sb("oc", [128, 8, T], BF16)
            SQ = [sb(f"sq{i}", [128, T], BF16) for i in range(2)]
            RS = sb("rs", [128, T], F32)
            SQ8 = sb("sq8", [128, 8, T], BF16)
            CG = [sb(f"cg{i}", [128, T], F32) for i in range(2)]
            ZB = sb("zb", [128, 4, T + 2], F32)
            AC = [sb(f"ac{i}", [128, T], F32) for i in range(2)]
            UB = sb("ub", [128, 4, T + 16], F32)
            SA = [sb(f"sa{i}", [128, T + 16], F32) for i in range(4)]
            PL = [sb(f"pl{i}", [128, T], BF16) for i in range(2)]
            ICN = sb("icn", [128, 4, 16], F32)
            TMP = sb("tmp16", [128, 16], F32)
            self.load_x(XTs[0], src, 0, T)
            wi = self.dr["odd_w_in"]
            wo = self.dr["w_o"]
            for kc in range(8):
                S.pool.dma_start(out=WI[:, kc, :], in_=wi[j, kc * 128:(kc + 1) * 128, :])
            S.pool.dma_start(out=WO[:, :, :], in_=wo[L].rearrange("(a p) n -> p a n", p=128))
            S.pool.dma_start(out=PW[:, :, :], in_=self.dr["poolw"][j])
            S.sp.dma_start(out=CW[:, :, :], in_=self.dr["convw"][:, j])
            S.sp.dma_start(out=PSC[:, :], in_=self.dr["poolscale"][:, j])
            S.dve.memset(ap=ZB[:, :, 0:2], constant=0.0)
            S.dve.memset(ap=UB[:, :, 0:16], constant=0.0)
            for g, w in enumerate(WIN):
                S.dve.memset(ap=ICN[:, g, :], constant=1.0 / w)
                for t in range(w - 1):
                    S.dve.memset(ap=ICN[:, g, t:t + 1], constant=1.0 / (t + 1))
            self.rmsnorm_T(XTs[0], HTs[0], SQ, RS, L, T)
            for it in range(NT):
                t0 = it * T
                XT = XTs[it % 2]
                HT = HTs[it % 2]
                if it + 1 < NT:
                    self.load_x(XTs[(it + 1) % 2], src, t0 + T, T)

                def proj(col):
                    pb = self.bank()
                    for kc in range(8):
                        S.pe.matmul(out=pb[:, :], lhsT=WI[:, kc, col * 128:(col + 1) * 128], rhs=HT[:, kc, :],
                                    start=(kc == 0), stop=(kc == 7))
                    return pb

                def pool_head(g):
                    w = WIN[g]
                    p_u = proj(12 + g)
                    S.act.copy(out=UB[:, g, 16:W_], in_=p_u[:, :])
                    cur = UB[:, g, :]
                    sh = 1
                    lo = 0
                    k = 0
                    nst = {2: 1, 4: 2, 8: 3, 16: 4}[w]
                    while sh < w:
                        nlo = lo + sh
                        o = SA[2 + g % 2] if k == nst - 1 else SA[k % 2]
                        S.pool.tensor_tensor(out=o[:, nlo:W_], in0=cur[:, nlo:W_], in1=cur[:, nlo - sh:W_ - sh], op=ALU.add)
                        cur = o
                        lo = nlo
                        sh *= 2
                        k += 1
                    return cur

                def pool_tail(g, cur):
                    w = WIN[g]
                    pl = PL[g % 2]
                    S.dve.scalar_tensor_tensor(out=pl[:, :], in0=cur[:, 16:W_], scalar=1.0 / w, in1=UB[:, g, 16:W_],
                                               op0=ALU.mult, op1=ALU.subtract)
                    if it == 0:
                        S.dve.tensor_tensor(out=TMP[:, :], in0=cur[:, 16:32], in1=ICN[:, g, :], op=ALU.mult)
                        S.dve.tensor_tensor(out=pl[:, 0:16], in0=TMP[:, :], in1=UB[:, g, 16:32], op=ALU.subtract)
                    S.pool.tensor_copy(out=UB[:, g, 0:16], in_=UB[:, g, T:T + 16])
                    pb = self.bank()
                    S.pe.matmul(out=pb[:, :], lhsT=PW[:, g, :], rhs=pl[:, :], start=True, stop=True)
                    S.act.activation(out=OC[:, 4 + g, :], in_=pb[:, :], func=AF.Copy, scale=PSC[:, g:g + 1])

                def conv_chunk(c):
                    cg = CG[c % 2]
                    ac = AC[c % 2]
                    p_cg = proj(4 + c)
                    S.act.copy(out=cg[:, :], in_=p_cg[:, :])
                    p_xc = proj(8 + c)
                    S.dve.tensor_tensor(out=ZB[:, c, 2:T + 2], in0=p_xc[:, :], in1=cg[:, :], op=ALU.mult)
                    p_bg = proj(c)
                    S.dve.tensor_scalar(out=ac[:, :], in0=ZB[:, c, 2:T + 2], scalar1=CW[:, c, 2:3], scalar2=None,
                                        op0=ALU.mult)
                    S.dve.scalar_tensor_tensor(out=ac[:, :], in0=ZB[:, c, 1:T + 1], scalar=CW[:, c, 1:2], in1=ac[:, :],
                                               op0=ALU.mult, op1=ALU.add)
                    S.dve.scalar_tensor_tensor(out=ac[:, :], in0=ZB[:, c, 0:T], scalar=CW[:, c, 0:1], in1=ac[:, :],
                                               op0=ALU.mult, op1=ALU.add)
                    S.dve.tensor_tensor(out=OC[:, c, :], in0=ac[:, :], in1=p_bg[:, :], op=ALU.mult)
                    S.pool.tensor_copy(out=ZB[:, c, 0:2], in_=ZB[:, c, T:T + 2])

                prev = None
                for c in range(4):
                    cur = pool_head(c)
                    conv_chunk(c)
                    if prev is not None:
                        pool_tail(*prev)
                    prev = (c, cur)
                    if c == 1 and it + 1 < NT:
                        for cc in range(8):
                            S.act.activation(out=SQ8[:, cc, :], in_=XTs[(it + 1) % 2][:, cc, :], func=AF.Square)
                pool_tail(*prev)
                if it + 1 < NT:
                    XN = XTs[(it + 1) % 2]
                    HN = HTs[(it + 1) % 2]
                    pn = self.bank()
                    for cc in range(8):
                        S.pe.matmul(out=pn[:, :], lhsT=self.ones_bf[:], rhs=SQ8[:, cc, :], start=(cc == 0), stop=(cc == 7))
                    S.act.activation(out=RS[:, :], in_=pn[:, :], func=AF.Ln, bias=self.epsc[:], scale=1.0 / D)
                    S.act.activation(out=RS[:, :], in_=RS[:, :], func=AF.Exp, scale=-0.5)
                    for cc in range(8):
                        S.dve.scalar_tensor_tensor(out=HN[:, cc, :], in0=XN[:, cc, :], scalar=self.gains[:, L, cc:cc + 1],
                                                   in1=RS[:, :], op0=ALU.mult, op1=ALU.mult)
                for dc in range(8):
                    pb = self.bank()
                    for kc in range(8):
                        S.pe.matmul(out=pb[:, :], lhsT=WO[:, kc, dc * 128:(dc + 1) * 128], rhs=OC[:, kc, :],
                                    start=(kc == 0), stop=(kc == 7))
                    S.dve.tensor_tensor(out=XT[:, dc, :], in0=XT[:, dc, :], in1=pb[:, :], op=ALU.add)
                    self.store_x_chunk(XT, dst, t0, T, dc)
            S.barrier()

    def even_phase(self, L, src, dst, pid):
        S = self.S
        nc = self.nc
        j = L // 2
        T = 256
        NT = self.seq // T
        NKT = self.seq // 128
        PB = self.PB
        PH = self.PH
        C_QA, C_KA, C_VA, C_RA, C_GL, C_QB, C_KB, C_VB = 0, 256, 512, 1024, 1536, 1552, 2064, 2576
        with ExitStack() as es:
            def sb(name, shape, dt):
                return es.enter_context(nc.sbuf_tensor(f"{name}_{pid}", list(shape), dt))
            WI = sb("wi", [128, 8, 3088], BF16)
            WO = sb("wo", [128, 8, D], BF16)
            KT = [sb(f"kt{h}", [128, self.seq], BF16) for h in range(4)]
            VA = sb("vaug", [128, NKT, 8, 65], BF16)
            XTs = [sb(f"xt{i}", [128, 8, T], F32) for i in range(2)]
            HT = sb("ht", [128, 8, T], BF16)
            OC = sb("oc", [128, 8, T], BF16)
            SQ = [sb(f"sq{i}", [128, T], BF16) for i in range(2)]
            RS = sb("rs", [128, T], F32)
            RS2 = [sb("rs20", [128, T], F32)] * 2
            SQ8 = sb("sq8", [128, 8, T], BF16)
            IDB = sb("idb", [128, 128], BF16)
            TRU = sb("tru", [128, 128], BF16)
            TRL = sb("trl", [128, 128], BF16)
            CM = sb("cm", [128, 512], BF16)
            BO = sb("bo", [128, 128], BF16)
            OH = sb("oh", [128, 16, 128], BF16)
            OWN = sb("own", [128, 8, 2, 256], BF16)
            ALB = sb("alb", [128, 8, 32], F32)
            ALQ = sb("alq", [128, 2, 8], F32)
            WG = sb("wg", [17, 256], BF16)
            GG = sb("gg", [128, 1], F32)
            QN = sb("qn", [128, 1], F32)
            KN = sb("kn", [128, 1], F32)
            GQK = sb("gqk", [128, 1], F32)
            QAT = sb("qat", [128, 2, T], BF16)
            KAT = sb("kat", [128, 2, T], BF16)
            GLR = sb("glr", [17, T], BF16)
            G_ = sb("g", [128, 256], F32)
            GH = sb("gh", [128, 256], BF16)
            GL_ = sb("gl", [128, 256], BF16)
            EB = sb("eb", [128, 128], F32)
            ENB = sb("enb", [128, 128], F32)
            DEC = sb("dec", [128, 2], F32)
            QS = sb("qs", [128, 4, 128], BF16)
            KS = sb("ks", [128, 2, 128], BF16)
            ATM = sb("atm", [128, 512], BF16)
            EXE = sb("exe", [128, 256], F32)
            KEND = sb("kend", [128, 256], BF16)
            KATK = sb("katk", [128, 256], F32)
            VTOK = sb("vtok", [128, 512], BF16)
            SIL = sb("sil", [128, 512], F32)
            OAT = sb("oat", [128, 512], BF16)
            ST = sb("st", [128, 2, 128], F32)
            STB = sb("stb", [128, 2, 256], BF16)
            SSQ = sb("ssq", [128, 4], F32)
            QT = [sb(f"qt{h}", [128, T], BF16) for h in range(8)]
            KM = [sb(f"km{h}", [128, 16], BF16) for h in range(4)]
            KMF = sb("kmf", [128, 1], F32)
            GSB = sb("gsb", [128, 2, 8, 16], F32)
            M3 = sb("m3", [128, 16], F32)
            GT = [sb(f"gt{i}", [128, 16, 16], F32) for i in range(2)]
            MB = sb("mb", [128, 2, 8, 16], BF16)
            MBT = sb("mbt", [128, 8, T], BF16)
            PT = [sb(f"pt{i}", [128, T], BF16) for i in range(3)]
            RD = sb("rd", [128, 2], F32)
            OBT = sb("obt", [128, 2, 512], BF16)

            self.load_x(XTs[0], src, 0, T)
            wi = self.dr["even_w_in"]
            wo = self.dr["w_o"]
            for kc in range(8):
                S.pool.dma_start(out=WI[:, kc, :], in_=wi[j, kc * 128:(kc + 1) * 128, :])
            S.pool.dma_start(out=WO[:, :, :], in_=wo[L].rearrange("(a p) n -> p a n", p=128))
            S.pool.dma_start(out=IDB[:], in_=self.dr["ident"])
            S.pool.dma_start(out=TRU[:], in_=self.dr["triu"])
            S.pool.dma_start(out=TRL[:], in_=self.dr["tril"])
            S.pool.dma_start(out=CM[:], in_=self.dr["cmask"])
            S.pool.dma_start(out=BO[:], in_=self.dr["blockones"])
            S.pool.dma_start(out=OH[:], in_=self.dr["onehot"])
            S.pool.dma_start(out=OWN[:], in_=self.dr["ownmask"])
            S.sp.dma_start(out=ALB[:], in_=self.dr["alibib"])
            S.sp.dma_start(out=ALQ[:], in_=self.dr["alibiq"])
            S.pool.dma_start(out=WG[:], in_=self.dr["wg_aug"][j])
            S.sp.dma_start(out=GG[:], in_=self.dr["gla_gain"][j])
            S.sp.dma_start(out=QN[:], in_=self.dr["qn"][j])
            S.sp.dma_start(out=KN[:], in_=self.dr["kn"][j])
            S.dve.scalar_tensor_tensor(out=GQK[:], in0=QN[:], scalar=0.125, in1=KN[:], op0=ALU.mult, op1=ALU.mult)
            S.dve.memset(ap=GLR[:, :], constant=1.0)
            S.dve.memset(ap=ST[:, :, :], constant=0.0)
            S.dve.memset(ap=STB[:, :, :], constant=0.0)
            S.dve.memset(ap=QS[:, :, :], constant=0.0)
            S.dve.memset(ap=MBT[:, :, :], constant=0.0)
            for h in range(8):
                S.dve.memset(ap=QT[h][:, :], constant=0.0)
            S.dve.memset(ap=VA[:, :, :, :].rearrange("p k h c -> p (k h c)"), constant=1.0)
            for h in range(4):
                S.dve.memset(ap=KM[h][:, :], constant=0.0)

            def proj_f(col0, M):
                pb = self.bank(0, 4)
                for kc in range(8):
                    S.pe.matmul(out=pb[0:M, 0:T], lhsT=WI[:, kc, col0:col0 + M], rhs=HT[:, kc, 0:T],
                                start=(kc == 0), stop=(kc == 7))
                return pb

            def proj_t(ts, col0, N):
                pb = self.bank(0, 4)
                for kc in range(8):
                    S.pe.matmul(out=pb[:, 0:N], lhsT=HT[:, kc, ts * 128:(ts + 1) * 128], rhs=WI[:, kc, col0:col0 + N],
                                start=(kc == 0), stop=(kc == 7))
                return pb

            for it in range(NT):
                t0 = it * T
                b = it
                XT = XTs[it % 2]
                if it + 1 < NT:
                    self.load_x(XTs[(it + 1) % 2], src, t0 + T, T)
                if it == 0:
                    self.rmsnorm_T(XT, HT, SQ, RS, L, T)
                def qk_finish(hp, which, pb, sq):
                    p2 = self.bank(4, 7)
                    S.pe.matmul(out=p2[:, 0:T], lhsT=BO[:], rhs=sq[:, 0:T], start=True, stop=True)
                    r2 = RS2[hp % 2]
                    S.act.activation(out=r2[:, :], in_=p2[:, 0:T], func=AF.Ln, bias=self.epsc[:], scale=1.0 / 64)
                    S.act.activation(out=r2[:, :], in_=r2[:, :], func=AF.Exp, scale=-0.5)
                    if which == 0:
                        for e in range(2):
                            r0 = e * 64
                            S.dve.scalar_tensor_tensor(out=QT[2 * hp + e][r0:r0 + 64, :], in0=pb[r0:r0 + 64, 0:T],
                                                       scalar=GQK[r0:r0 + 64, 0:1], in1=r2[r0:r0 + 64, :],
                                                       op0=ALU.mult, op1=ALU.mult)
                    else:
                        S.dve.tensor_tensor(out=KT[hp][:, t0:t0 + T], in0=pb[:, 0:T], in1=r2[:, :], op=ALU.mult)
                        S.dve.tensor_reduce(out=KMF[:, :], in_=KT[hp][:, t0:t0 + T], axis=AX.X, op=ALU.add)
                        S.dve.tensor_copy(out=KM[hp][:, b:b + 1], in_=KMF[:, :])
                def qk_items(which):
                    prevqk = None
                    for hp in range(4):
                        pb = proj_f((C_QB if which == 0 else C_KB) + hp * 128, 128)
                        sq = SQ[hp % 2]
                        S.act.activation(out=sq[:, 0:T], in_=pb[:, 0:T], func=AF.Square)
                        if prevqk is not None:
                            qk_finish(*prevqk)
                        prevqk = (hp, which, pb, sq)
                    qk_finish(*prevqk)

                def kv_items():
                    qk_items(1)
                    for ts in range(2):
                        pb = proj_t(ts, C_VB, 512)
                        if ts == 0:
                            S.act.copy(out=VA[:, it * 2 + ts, :, 0:64], in_=pb[:, :].rearrange("p (h c) -> p h c", c=64))
                        else:
                            S.dve.tensor_copy(out=VA[:, it * 2 + ts, :, 0:64], in_=pb[:, :].rearrange("p (h c) -> p h c", c=64))

                qk_items(0)
                if b >= 1:
                    pg = self.bank(4, 7)
                    for ts in range(2):
                        for h in range(8):
                            hp, r0 = h // 2, (h % 2) * 64
                            c0 = (ts * 8 + h) * 16
                            S.pe.matmul(out=pg[:, c0:c0 + 16], lhsT=QT[h][:, ts * 128:(ts + 1) * 128],
                                        rhs=KM[hp][:, :], start=True, stop=True)
                    S.dve.memset(ap=GSB[:, :, :, :], constant=-1e30)
                    S.dve.tensor_copy(out=GSB[:, :, :, 0:b],
                                      in_=pg[:, 0:256].rearrange("p (t h n) -> p t h n", t=2, h=8)[:, :, :, 0:b])
                    G3 = GSB[:, :, :, :].rearrange("p t h n -> p (t h) n")
                    cur = G3
                    for r in range(3):
                        S.dve.tensor_reduce(out=M3[:, :], in_=cur, axis=AX.X, op=ALU.max)
                        if r == 2:
                            break
                        nxt = GT[r]
                        S.dve.tensor_tensor(out=nxt[:, :, :], in0=cur, in1=M3[:, :].unsqueeze(2).to_broadcast([128, 16, 16]),
                                            op=ALU.is_ge)
                        S.dve.scalar_tensor_tensor(out=nxt[:, :, :], in0=nxt[:, :, :], scalar=-2e30, in1=cur,
                                                   op0=ALU.mult, op1=ALU.add)
                        cur = nxt[:, :, :]
                    S.dve.tensor_tensor(out=GT[0][:, :, :], in0=G3, in1=M3[:, :].unsqueeze(2).to_broadcast([128, 16, 16]),
                                        op=ALU.is_lt)
                    S.dve.tensor_scalar(out=MB[:, :, :, :].rearrange("p t h n -> p (t h) n"), in0=GT[0][:, :, :],
                                        scalar1=NEG, scalar2=None, op0=ALU.mult)
                    S.dve.tensor_copy(out=MB[:, :, :, 15:16], in_=ALQ[:, :, :].rearrange("p t (h o) -> p t h o", o=1))
                kv_items()
                if b >= 1:
                    for ts in range(2):
                        for h in range(8):
                            S.pe.transpose(out=PH[0:16, h * 128:(h + 1) * 128], in_=MB[:, ts, h, :], identity=IDB[:])
                        S.act.copy(out=MBT[0:16, :, ts * 128:(ts + 1) * 128],
                                   in_=PH[0:16, :].rearrange("p (h q) -> p h q", h=8))
                def gla_gen():
                    gcnt = [0]

                    def gbank():
                        bk = PB[5 + gcnt[0] % 2]
                        gcnt[0] += 1
                        return bk

                    def gproj_f(col0, M):
                        pb = gbank()
                        for kc in range(8):
                            S.pe.matmul(out=pb[0:M, 0:T], lhsT=WI[:, kc, col0:col0 + M], rhs=HT[:, kc, 0:T],
                                        start=(kc == 0), stop=(kc == 7))
                        return pb

                    def gproj_t(ts, col0, N):
                        pb = gbank()
                        for kc in range(8):
                            S.pe.matmul(out=pb[:, 0:N], lhsT=HT[:, kc, ts * 128:(ts + 1) * 128], rhs=WI[:, kc, col0:col0 + N],
                                        start=(kc == 0), stop=(kc == 7))
                        return pb

                    for hp in range(2):
                        pb = gproj_f(C_QA + hp * 128, 128)
                        S.dve.tensor_copy(out=QAT[:, hp, :], in_=pb[:, 0:T])
                        yield
                        pb = gproj_f(C_KA + hp * 128, 128)
                        S.dve.tensor_copy(out=KAT[:, hp, :], in_=pb[:, 0:T])
                        yield
                    pb = gproj_f(C_GL, 16)
                    S.dve.tensor_copy(out=GLR[0:16, :], in_=pb[0:16, 0:T])
                    yield
                    for ts in range(2):
                        tc0 = ts * 128
                        pb = gproj_t(ts, C_VA, 512)
                        S.dve.tensor_copy(out=VTOK[:, :], in_=pb[:, :])
                        yield
                        pb = gproj_t(ts, C_RA, 512)
                        S.act.activation(out=SIL[:, :], in_=pb[:, :], func=AF.Exp, scale=-1.0)
                        yield
                        S.act.activation(out=SIL[:, :], in_=SIL[:, :], func=AF.Ln, bias=1.0, scale=1.0)
                        S.act.activation(out=SIL[:, :], in_=SIL[:, :], func=AF.Exp, scale=-1.0)
                        S.dve.tensor_tensor(out=SIL[:, :], in0=SIL[:, :], in1=pb[:, :], op=ALU.mult)
                        yield
                        pb = gproj_t(ts, C_KA, 256)
                        S.dve.tensor_copy(out=KATK[:, :], in_=pb[:, 0:256])
                        yield
                        pz = gbank()
                        S.pe.matmul(out=pz[:, 0:256], lhsT=GLR[:, tc0:tc0 + 128], rhs=WG[:, :], start=True, stop=True)
                        S.act.activation(out=G_[:, :], in_=pz[:, 0:256], func=AF.Exp, scale=-1.0)
                        S.act.activation(out=G_[:, :], in_=G_[:, :], func=AF.Ln, bias=1.0, scale=1.0)
                        S.dve.tensor_copy(out=GH[:, :], in_=G_[:, :])
                        S.dve.tensor_tensor(out=GL_[:, :], in0=G_[:, :], in1=GH[:, :], op=ALU.subtract)
                        yield
                        pe_ = gbank()
                        S.pe.matmul(out=pe_[:, 0:256], lhsT=TRL[:, :], rhs=GH[:, :], start=True, stop=False)
                        S.pe.matmul(out=pe_[:, 0:256], lhsT=TRL[:, :], rhs=GL_[:, :], start=False, stop=True)
                        S.act.activation(out=EXE[:, :], in_=pe_[:, 0:256], func=AF.Exp)
                        S.dve.tensor_tensor(out=KEND[:, :], in0=KATK[:, :], in1=EXE[:, :], op=ALU.mult)
                        yield
                        for hp in range(2):
                            pbt = gbank()
                            S.pe.matmul(out=pbt[:, 0:128], lhsT=GH[:, hp * 128:(hp + 1) * 128], rhs=TRU[:, :],
                                        start=True, stop=False)
                            S.pe.matmul(out=pbt[:, 0:128], lhsT=GL_[:, hp * 128:(hp + 1) * 128], rhs=TRU[:, :],
                                        start=False, stop=True)
                            S.act.activation(out=EB[:, :], in_=pbt[:, 0:128], func=AF.Exp)
                            S.act.activation(out=ENB[:, :], in_=pbt[:, 0:128], func=AF.Exp, scale=-1.0)
                            yield
                            S.dve.tensor_copy(out=DEC[:, hp:hp + 1], in_=EB[:, 127:128])
                            for e in range(2):
                                r0 = e * 64
                                S.dve.scalar_tensor_tensor(out=QS[r0:r0 + 64, 2 * hp + e, :], in0=QAT[r0:r0 + 64, hp, tc0:tc0 + 128],
                                                           scalar=0.125, in1=EB[r0:r0 + 64, :], op0=ALU.mult, op1=ALU.mult)
                            S.dve.tensor_tensor(out=KS[:, hp, :], in0=KAT[:, hp, tc0:tc0 + 128], in1=ENB[:, :], op=ALU.mult)
                            yield
                        for hp in range(2):
                            pds = gbank()
                            S.pe.matmul(out=pds[:, 0:256], lhsT=KEND[:, hp * 128:(hp + 1) * 128],
                                        rhs=VTOK[:, hp * 256:(hp + 1) * 256], start=True, stop=True)
                            for e in range(2):
                                r0 = e * 64
                                S.dve.scalar_tensor_tensor(out=ST[r0:r0 + 64, hp, :], in0=ST[r0:r0 + 64, hp, :],
                                                           scalar=DEC[r0:r0 + 64, hp:hp + 1],
                                                           in1=pds[r0:r0 + 64, e * 128:(e + 1) * 128],
                                                           op0=ALU.mult, op1=ALU.add)
                            yield
                        pat = gbank()
                        for h in range(4):
                            S.pe.matmul(out=pat[:, h * 128:(h + 1) * 128], lhsT=KS[:, h // 2, :],
                                        rhs=QS[:, h, :], start=True, stop=True)
                        S.dve.tensor_tensor(out=ATM[:, :], in0=pat[:, :], in1=CM[:, :], op=ALU.mult)
                        yield
                        po = gbank()
                        for h in range(4):
                            hp = h // 2
                            S.pe.matmul(out=po[:, h * 128:(h + 1) * 128], lhsT=ATM[:, h * 128:(h + 1) * 128],
                                        rhs=VTOK[:, h * 128:(h + 1) * 128], start=(h == 0), stop=False,
                                        skip_group_check=True)
                            S.pe.matmul(out=po[:, hp * 256:(hp + 1) * 256], lhsT=QS[:, h, :],
                                        rhs=STB[:, hp, :], start=False, stop=True, skip_group_check=True)
                        yield
                        for e in range(2):
                            r0 = e * 64
                            S.pool.tensor_copy(out=STB[r0:r0 + 64, :, e * 128:(e + 1) * 128], in_=ST[r0:r0 + 64, :, :])
                        for h in range(4):
                            S.act.activation(out=OAT[:, h * 128:(h + 1) * 128], in_=po[:, h * 128:(h + 1) * 128], func=AF.Square,
                                             accum_out=SSQ[:, h:h + 1])
                        yield
                        S.act.activation(out=SSQ[:, :], in_=SSQ[:, :], func=AF.Ln, bias=self.epsc[:], scale=1.0 / 128)
                        S.act.activation(out=SSQ[:, :], in_=SSQ[:, :], func=AF.Exp, scale=-0.5)
                        for h in range(4):
                            S.dve.scalar_tensor_tensor(out=OAT[:, h * 128:(h + 1) * 128], in0=po[:, h * 128:(h + 1) * 128],
                                                       scalar=SSQ[:, h:h + 1], in1=SIL[:, h * 128:(h + 1) * 128],
                                                       op0=ALU.mult, op1=ALU.mult)
                        yield
                        for h in range(4):
                            S.pe.transpose(out=PH[:, h * 128:(h + 1) * 128], in_=OAT[:, h * 128:(h + 1) * 128], identity=IDB[:])
                        for h in range(4):
                            S.dve.tensor_scalar(out=OC[:, h, tc0:tc0 + 128], in0=PH[:, h * 128:(h + 1) * 128],
                                                scalar1=GG[:, 0:1], scalar2=None, op0=ALU.mult)
                        yield
                nkt = 2 * b + 2
                PAST_CAP = (1, 2, 4, 8, 16, 16, 16, 16)
                kt_lo = [max(0, 2 * (b - PAST_CAP[h])) for h in range(8)]
                items = [(h, kt) for h in range(8) for kt in range(kt_lo[h], nkt)]
                first_of = {}
                scnt = [0]

                def att_s1(h, kt):
                    hp = h // 2
                    ps = PB[scnt[0] % 3]
                    scnt[0] += 1
                    S.pe.matmul(out=ps[:, 0:T], lhsT=KT[hp][:, kt * 128:(kt + 1) * 128],
                                rhs=QT[h][:, :], start=True, stop=False)
                    if kt < 2 * b:
                        S.pe.matmul(out=ps[:, 0:T], lhsT=OH[:, kt // 2, :], rhs=MBT[:, h, :], start=False, stop=True)
                    else:
                        S.pe.matmul(out=ps[:, 0:T], lhsT=IDB[:, :], rhs=OWN[:, h, kt - 2 * b, :], start=False, stop=True)
                    return ps

                def att_s23(h, kt, ps, cnt):
                    pacc = PB[3 + (h % 2)]
                    pt = PT[cnt % 3]
                    if kt < 2 * b:
                        dl = 2 * b - kt
                        S.act.activation(out=pt[:, :], in_=ps[:, 0:T], func=AF.Exp, bias=ALB[:, h, dl:dl + 1], scale=1.0)
                    else:
                        S.act.activation(out=pt[:, :], in_=ps[:, 0:T], func=AF.Exp)
                    for ts in range(2):
                        if kt == 2 * b + 1 and ts == 0:
                            continue
                        S.pe.matmul(out=pacc[:, ts * 65:(ts + 1) * 65], lhsT=pt[:, ts * 128:(ts + 1) * 128],
                                    rhs=VA[:, kt, h, :], start=(h not in first_of), stop=(kt == nkt - 1),
                                    skip_group_check=True)
                        first_of[h] = True
                    if kt == nkt - 1:
                        S.dve.reciprocal(out=RD[:, :], in_=pacc[:, 0:130].rearrange("p (t c) -> p t c", c=65)[:, :, 64])
                        for ts in range(2):
                            S.dve.tensor_scalar(out=OBT[:, ts, h * 64:(h + 1) * 64], in0=pacc[:, ts * 65:ts * 65 + 64],
                                                scalar1=RD[:, ts:ts + 1], scalar2=None, op0=ALU.mult)

                if it + 1 < NT:
                    XN = XTs[(it + 1) % 2]
                    for c in range(8):
                        S.act.activation(out=SQ8[:, c, :], in_=XN[:, c, 0:T], func=AF.Square)
                gen = gla_gen()
                gen_done = [False]

                def gla_step(k):
                    for _ in range(k):
                        if gen_done[0]:
                            return
                        try:
                            next(gen)
                        except StopIteration:
                            gen_done[0] = True

                NUNITS = 50
                pend = []
                cnt = 0
                done_units = 0
                for ii, (h, kt) in enumerate(items):
                    pend.append((h, kt, att_s1(h, kt)))
                    if len(pend) > 2:
                        h0, k0, ps0 = pend.pop(0)
                        att_s23(h0, k0, ps0, cnt)
                        cnt += 1
                    want = -(-(ii + 1) * NUNITS // len(items))
                    gla_step(want - done_units)
                    done_units = want
                while pend:
                    h0, k0, ps0 = pend.pop(0)
                    att_s23(h0, k0, ps0, cnt)
                    cnt += 1
                gla_step(10 ** 6)
                if it + 1 < NT:
                    pn = PB[5]
                    for c in range(8):
                        S.pe.matmul(out=pn[:, 0:T], lhsT=self.ones_bf[:], rhs=SQ8[:, c, :], start=(c == 0), stop=(c == 7))
                    S.act.activation(out=RS[:, 0:T], in_=pn[:, 0:T], func=AF.Ln, bias=self.epsc[:], scale=1.0 / D)
                    S.act.activation(out=RS[:, 0:T], in_=RS[:, 0:T], func=AF.Exp, scale=-0.5)
                    for c in range(8):
                        S.dve.scalar_tensor_tensor(out=HT[:, c, 0:T], in0=XN[:, c, 0:T], scalar=self.gains[:, L, c:c + 1],
                                                   in1=RS[:, 0:T], op0=ALU.mult, op1=ALU.mult)
                for ts in range(2):
                    for hp in range(4):
                        S.pe.transpose(out=PH[:, hp * 128:(hp + 1) * 128], in_=OBT[:, ts, hp * 128:(hp + 1) * 128],
                                       identity=IDB[:])
                    S.dve.tensor_copy(out=OC[:, 4:8, ts * 128:(ts + 1) * 128],
                                      in_=PH[:, 0:512].rearrange("p (h q) -> p h q", h=4))
                for dc in range(8):
                    pb = self.bank(0, 5)
                    for kc in range(8):
                        S.pe.matmul(out=pb[:, 0:T], lhsT=WO[:, kc, dc * 128:(dc + 1) * 128], rhs=OC[:, kc, :],
                                    start=(kc == 0), stop=(kc == 7))
                    S.dve.tensor_tensor(out=XT[:, dc, :], in0=XT[:, dc, :], in1=pb[:, 0:T], op=ALU.add)
                    self.store_x_chunk(XT, dst, t0, T, dc)
            S.barrier()


FULL_PLAN = []
for _l in range(DEPTH):
    FULL_PLAN.append(("even" if _l % 2 == 0 else "odd", _l))
    FULL_PLAN.append(("mlp", _l))


def prep_params(inp):
    p = {}
    g = np.concatenate([np.asarray(inp["norm_mix"], np.float32), np.asarray(inp["norm_mlp"], np.float32)], axis=0)
    p["gains"] = np.ascontiguousarray(g.reshape(8, 8, 128).transpose(2, 0, 1))
    for k in ("w_o", "w1", "w2", "even_w_in", "odd_w_in"):
        p[k] = np.ascontiguousarray(np.asarray(inp[k], np.float32))
    p["wg_aug"] = np.ascontiguousarray(np.concatenate(
        [np.asarray(inp["gla_w_gate2"], np.float32), np.asarray(inp["gla_b_gate"], np.float32)[:, None, :]], axis=1))
    p["gla_gain"] = np.ascontiguousarray(np.asarray(inp["gla_out_norm"], np.float32)[:, :, None])
    p["qn"] = np.ascontiguousarray(np.tile(np.asarray(inp["moba_q_norm"], np.float32), (1, 2))[:, :, None])
    p["kn"] = np.ascontiguousarray(np.tile(np.asarray(inp["moba_k_norm"], np.float32), (1, 2))[:, :, None])
    cw = np.asarray(inp["conv_w"], np.float32)
    p["convw"] = np.ascontiguousarray(cw.reshape(2, 3, 4, 128).transpose(3, 0, 2, 1))
    ps = np.asarray(inp["pool_scale"], np.float32)
    p["poolscale"] = np.ascontiguousarray(ps.reshape(2, 4, 128).transpose(2, 0, 1))
    pw = np.asarray(inp["pool_w"], np.float32)
    p["poolw"] = np.ascontiguousarray(pw.transpose(0, 2, 1, 3))
    return p


_CACHE = {}


def run_plan(plan, inputs, x_list, seq=SEQ):
    key = (tuple(plan), seq)
    if key not in _CACHE:
        _CACHE[key] = Builder(plan, seq)
    bld = _CACHE[key]
    base = dict(host_constants())
    base.update(prep_params(inputs))
    in_maps = []
    for xb in x_list:
        m = dict(base)
        m["xT"] = np.ascontiguousarray(np.asarray(xb, np.float32).T)
        in_maps.append(m)
    res = run_bass_kernel_spmd(bld.nc, in_maps, core_ids=list(range(len(x_list))))
    return [np.ascontiguousarray(r["yT"].T) for r in res.results]


def kernel(**inputs):
    x = np.asarray(inputs["x"], np.float32)
    outs = run_plan(FULL_PLAN, inputs, [x[b] for b in range(x.shape[0])])
    return np.stack(outs, axis=0).astype(np.float32)
```
